# Optimizing a Trainium2 kernel written in Bass

```python
import jax, jax.numpy as jnp
from jax import lax
import numpy as np

D_MODEL = 1024
BATCH = 8
SEQ = 4096
DEPTH = 1

CHUNK = 64
D_MIX = D_MODEL
D_RWKV = D_MIX // 2
RWKV_HEAD = 64
RWKV_HEADS = D_RWKV // RWKV_HEAD
D_MLSTM = D_MIX - D_RWKV
MLSTM_HEADS = 4
MLSTM_HEAD = D_MLSTM // MLSTM_HEADS
DECAY_LORA = 64
ICL_LORA = 64
CONV_K = 4
NORM_EPS = 1e-6
RWKV_GN_EPS = 64e-5
MLSTM_LN_EPS = 1e-6

RWKV_SHIFT_SPLITS = (D_RWKV, D_RWKV, D_RWKV, DECAY_LORA, ICL_LORA)
D_RWKV_SHIFT = sum(RWKV_SHIFT_SPLITS)
REST_SPLITS = (D_RWKV,
               D_MLSTM, D_MLSTM, D_MLSTM, D_MLSTM,
               MLSTM_HEADS, MLSTM_HEADS,
               D_MLSTM)
D_IN = D_RWKV_SHIFT + sum(REST_SPLITS)

kernel_name = 'hymba_rwkv7_mlstm_adaln_block'


def _offsets(sizes):
    return [int(s) for s in np.cumsum(sizes)[:-1]]


def _rmsnorm(x, gain):
    xf = x.astype(jnp.float32)
    return xf * lax.rsqrt(jnp.mean(xf * xf, axis=-1, keepdims=True) + NORM_EPS) * gain


def _token_shift_lerp(u, mu):
    prev = jnp.pad(u, ((0, 0), (1, 0), (0, 0)))[:, :-1]
    return u + mu * (prev - u)


def _causal_dwconv(u, w, bias):
    K = w.shape[0]
    T = u.shape[1]
    up = jnp.pad(u, ((0, 0), (K - 1, 0), (0, 0)))
    out = bias
    for j in range(K):
        out = out + w[j] * up[:, j:j + T]
    return out


def _rwkv7_step(S, inp):
    r, w, k, v, kk, a = inp
    sa = jnp.einsum('bhvk,bhk->bhv', S, -kk)
    S = S * w[:, :, None, :] + sa[..., None] * (kk * a)[:, :, None, :] + v[..., None] * k[:, :, None, :]
    y = jnp.einsum('bhvk,bhk->bhv', S, r)
    return S, y


def _rwkv7_branch(u, z, mu, w_decay_up, w_decay0, w_icl_up, a0, k_k, k_a, r_k, gn_w, gn_b):
    B, T, _ = u.shape
    u = _token_shift_lerp(u.astype(jnp.float32), mu)
    r, k, v, wd, ad = jnp.split(u, _offsets(RWKV_SHIFT_SPLITS), axis=-1)
    w_log = -jax.nn.softplus(-(w_decay0 + jnp.tanh(wd) @ w_decay_up)) - 0.5
    decay = jnp.exp(-jnp.exp(w_log))
    a = jax.nn.sigmoid(a0 + ad @ w_icl_up)
    hd = lambda t: t.reshape(B, T, RWKV_HEADS, RWKV_HEAD)
    kk = hd(k * k_k)
    kk = kk / jnp.maximum(jnp.sqrt(jnp.sum(kk * kk, axis=-1, keepdims=True)), 1e-12)
    k = k * (1.0 + (a - 1.0) * k_a)
    r_h, w_h, k_h, v_h, a_h = hd(r), hd(decay), hd(k), hd(v), hd(a)
    xs = tuple(jnp.swapaxes(t, 0, 1) for t in (r_h, w_h, k_h, v_h, kk, a_h))
    S0 = jnp.zeros((B, RWKV_HEADS, RWKV_HEAD, RWKV_HEAD), jnp.float32)
    _, y = lax.scan(_rwkv7_step, S0, xs)
    y = jnp.swapaxes(y, 0, 1)
    mean = jnp.mean(y, axis=-1, keepdims=True)
    var = jnp.mean(jnp.square(y - mean), axis=-1, keepdims=True)
    y = ((y - mean) * lax.rsqrt(var + RWKV_GN_EPS)).reshape(B, T, D_RWKV) * gn_w + gn_b
    bonus = jnp.sum(r_h * k_h * r_k.reshape(RWKV_HEADS, RWKV_HEAD), axis=-1, keepdims=True) * v_h
    y = y + bonus.reshape(B, T, D_RWKV)
    return y * jax.nn.silu(z.astype(jnp.float32))


def _mlstm_chunk_step(carry, inp):
    C, n, m = carry
    q, k, v, ig, lf = inp
    L = q.shape[2]
    b = jnp.cumsum(lf, axis=-1)
    b_last = b[..., -1]
    causal = jnp.tril(jnp.ones((L, L), dtype=bool))
    d_log = jnp.where(causal, b[..., :, None] - b[..., None, :] + ig[..., None, :], -jnp.inf)
    inter_log = b + m[..., None]
    m_t = jnp.maximum(inter_log, jnp.max(d_log, axis=-1))
    d_w = jnp.exp(d_log - m_t[..., None])
    scores = jnp.einsum('bhtd,bhsd->bhts', q, k) * d_w
    inter = jnp.exp(inter_log - m_t)
    num = jnp.einsum('bhts,bhsd->bhtd', scores, v) + inter[..., None] * jnp.einsum('bhvk,bhtk->bhtv', C, q)
    den = jnp.sum(scores, axis=-1) + inter * jnp.einsum('bhk,bhtk->bht', n, q)
    h = num / jnp.maximum(jnp.abs(den), jnp.exp(-m_t))[..., None]
    g = b_last[..., None] - b + ig
    m_new = jnp.maximum(b_last + m, jnp.max(g, axis=-1))
    w_s = jnp.exp(g - m_new[..., None])
    carry_scale = jnp.exp(b_last + m - m_new)
    C = carry_scale[..., None, None] * C + jnp.einsum('bhs,bhsv,bhsk->bhvk', w_s, v, k)
    n = carry_scale[..., None] * n + jnp.einsum('bhs,bhsk->bhk', w_s, k)
    return (C, n, m_new), h


def _mlstm_branch(q, k, v, o, ig, fg, z, conv_w, conv_b, b_i, b_f, ln_w, skip):
    B, T, _ = q.shape
    qk = jax.nn.silu(_causal_dwconv(jnp.concatenate([q, k], axis=-1).astype(jnp.float32), conv_w, conv_b))
    q_c, k_c = jnp.split(qk, 2, axis=-1)
    nc = T // CHUNK
    def chunked(t):
        return t.reshape(B, nc, CHUNK, MLSTM_HEADS, MLSTM_HEAD).transpose(1, 0, 3, 2, 4)
    def chunked_gate(t):
        return t.reshape(B, nc, CHUNK, MLSTM_HEADS).transpose(1, 0, 3, 2)
    i_pre = ig.astype(jnp.float32) + b_i
    log_f = jax.nn.log_sigmoid(fg.astype(jnp.float32) + b_f)
    xs = (chunked(q_c), chunked(k_c) * (MLSTM_HEAD ** -0.5), chunked(v.astype(jnp.float32)),
          chunked_gate(i_pre), chunked_gate(log_f))
    carry0 = (jnp.zeros((B, MLSTM_HEADS, MLSTM_HEAD, MLSTM_HEAD), jnp.float32),
              jnp.zeros((B, MLSTM_HEADS, MLSTM_HEAD), jnp.float32),
              jnp.zeros((B, MLSTM_HEADS), jnp.float32))
    _, h = lax.scan(_mlstm_chunk_step, carry0, xs)
    h = h.transpose(1, 0, 3, 2, 4).reshape(B, T, MLSTM_HEADS, MLSTM_HEAD)
    mean = jnp.mean(h, axis=-1, keepdims=True)
    var = jnp.mean(jnp.square(h - mean), axis=-1, keepdims=True)
    h = ((h - mean) * lax.rsqrt(var + MLSTM_LN_EPS)).reshape(B, T, D_MLSTM) * ln_w
    h = jax.nn.sigmoid(o.astype(jnp.float32)) * h + skip * q_c
    return h * jax.nn.silu(z.astype(jnp.float32))


def setup_inputs(seed: int = 0) -> dict:
    key = jax.random.key(seed)
    ks = jax.random.split(key, 32)
    def nrm(k, shape, scale):
        return jax.random.normal(k, shape, jnp.float32) * scale
    x = nrm(ks[0], (BATCH, SEQ, D_MODEL), 1.0)
    c = nrm(ks[1], (BATCH, D_MODEL), 1.0)
    w_ada = nrm(ks[2], (DEPTH, D_MODEL, 3 * D_MODEL), D_MODEL ** -0.5)
    b_ada = nrm(ks[3], (DEPTH, 3 * D_MODEL), 0.02)
    norm_gain = 1.0 + nrm(ks[4], (DEPTH, D_MODEL), 0.02)
    w_in = nrm(ks[5], (DEPTH, D_MODEL, D_IN), D_MODEL ** -0.5)
    mu_rwkv = jax.random.uniform(ks[6], (DEPTH, D_RWKV_SHIFT), jnp.float32)
    w_decay_up = nrm(ks[7], (DEPTH, DECAY_LORA, D_RWKV), 0.5 * DECAY_LORA ** -0.5)
    w_decay0 = jnp.linspace(-6.5, -1.5, D_RWKV, dtype=jnp.float32)[None] + nrm(ks[8], (DEPTH, D_RWKV), 0.1)
    w_icl_up = nrm(ks[9], (DEPTH, ICL_LORA, D_RWKV), ICL_LORA ** -0.5)
    a0 = nrm(ks[10], (DEPTH, D_RWKV), 0.1)
    k_k = 0.85 + nrm(ks[11], (DEPTH, D_RWKV), 0.02)
    k_a = 1.0 + nrm(ks[12], (DEPTH, D_RWKV), 0.02)
    r_k = nrm(ks[13], (DEPTH, D_RWKV), 0.1)
    rwkv_gn_w = 1.0 + nrm(ks[14], (DEPTH, D_RWKV), 0.02)
    rwkv_gn_b = nrm(ks[15], (DEPTH, D_RWKV), 0.02)
    mlstm_conv_w = nrm(ks[16], (DEPTH, CONV_K, 2 * D_MLSTM), CONV_K ** -0.5)
    mlstm_conv_b = nrm(ks[17], (DEPTH, 2 * D_MLSTM), 0.02)
    mlstm_b_i = nrm(ks[18], (DEPTH, MLSTM_HEADS), 0.1)
    mlstm_b_f = jnp.linspace(3.0, 6.0, MLSTM_HEADS, dtype=jnp.float32)[None] + nrm(ks[19], (DEPTH, MLSTM_HEADS), 0.1)
    mlstm_ln_w = 1.0 + nrm(ks[20], (DEPTH, D_MLSTM), 0.02)
    mlstm_skip = 1.0 + nrm(ks[21], (DEPTH, D_MLSTM), 0.02)
    w_out = nrm(ks[22], (DEPTH, D_MIX, D_MODEL), D_MIX ** -0.5)
    final_gain = 1.0 + nrm(ks[23], (D_MODEL,), 0.02)
    return {'x': x, 'c': c, 'w_ada': w_ada, 'b_ada': b_ada, 'norm_gain': norm_gain, 'w_in': w_in,
            'mu_rwkv': mu_rwkv, 'w_decay_up': w_decay_up, 'w_decay0': w_decay0, 'w_icl_up': w_icl_up,
            'a0': a0, 'k_k': k_k, 'k_a': k_a, 'r_k': r_k, 'rwkv_gn_w': rwkv_gn_w, 'rwkv_gn_b': rwkv_gn_b,
            'mlstm_conv_w': mlstm_conv_w, 'mlstm_conv_b': mlstm_conv_b, 'mlstm_b_i': mlstm_b_i,
            'mlstm_b_f': mlstm_b_f, 'mlstm_ln_w': mlstm_ln_w, 'mlstm_skip': mlstm_skip,
            'w_out': w_out, 'final_gain': final_gain}


def reference(x, c, w_ada, b_ada, norm_gain, w_in, mu_rwkv, w_decay_up, w_decay0, w_icl_up,
              a0, k_k, k_a, r_k, rwkv_gn_w, rwkv_gn_b, mlstm_conv_w, mlstm_conv_b, mlstm_b_i,
              mlstm_b_f, mlstm_ln_w, mlstm_skip, w_out, final_gain):
    h_res = x.astype(jnp.float32)
    c_act = jax.nn.silu(c.astype(jnp.float32))
    for l in range(DEPTH):
        ada = c_act @ w_ada[l] + b_ada[l]
        shift, scale, gate = jnp.split(ada, 3, axis=-1)
        hn = _rmsnorm(h_res, norm_gain[l]) * (1.0 + scale[:, None]) + shift[:, None]
        proj = hn @ w_in[l]
        u_rwkv = proj[..., :D_RWKV_SHIFT]
        z_r, q_m, k_m, v_m, o_m, i_m, f_m, z_m = jnp.split(proj[..., D_RWKV_SHIFT:], _offsets(REST_SPLITS), axis=-1)
        y_r = _rwkv7_branch(u_rwkv, z_r, mu_rwkv[l], w_decay_up[l], w_decay0[l], w_icl_up[l], a0[l],
                            k_k[l], k_a[l], r_k[l], rwkv_gn_w[l], rwkv_gn_b[l])
        y_m = _mlstm_branch(q_m, k_m, v_m, o_m, i_m, f_m, z_m, mlstm_conv_w[l], mlstm_conv_b[l],
                            mlstm_b_i[l], mlstm_b_f[l], mlstm_ln_w[l], mlstm_skip[l])
        mix = jnp.concatenate([y_r, y_m], axis=-1) @ w_out[l]
        h_res = h_res + gate[:, None] * mix
    return _rmsnorm(h_res, final_gain).astype(x.dtype)
```

```python
import contextlib
import numpy as np
import concourse.bass as bass
import concourse.mybir as mybir

F32 = mybir.dt.float32
BF16 = mybir.dt.bfloat16
AF = mybir.ActivationFunctionType
ALU = mybir.AluOpType
AX = mybir.AxisListType

NDS = 16
EP = 3000


class Sched:
    def __init__(self):
        self.streams = {e: [] for e in ('pe', 'act', 'dve', 'pool', 'sp')}
        self.cnt = {e: 0 for e in ('pe', 'act', 'dve', 'pool')}
        self.known = {e: {} for e in self.streams}
        self.snap = {}
        self.acc = {}
        self.dma_n = 0
        self.dma_cnt = [0] * NDS
        self.tracked = set()

    def track(self, t):
        self.tracked.add(t.name)
        return t

    def _box(self, ap):
        t = ap.tensor
        row = 1
        for s in t.shape[1:]:
            row *= s
        if t.name.startswith('pb') or t.name.startswith('pt'):
            return (0, 128, 0, row)
        off = ap.offset
        p0 = off // row
        f0 = off % row
        apl = ap.ap
        ps, pc = apl[0]
        p1 = p0 + (pc - 1) * (ps // row if ps else 0) + 1
        ext = 1
        for s, c in apl[1:]:
            ext += (c - 1) * abs(s)
        return (p0, p1, f0, f0 + ext)

    def _deps(self, ap, is_write, eng, deps):
        name = ap.tensor.name
        if name not in self.tracked:
            return
        b = self._box(ap)
        psum = name.startswith('pb') or name.startswith('pt')
        for r in self.acc.get(name, ()):
            if r[0] >= b[1] or b[0] >= r[1] or r[2] >= b[3] or b[2] >= r[3]:
                continue
            if not is_write and not r[5]:
                if not (psum and r[6] != eng):
                    continue
            if r[6] == eng and eng != 'sp':
                if eng == 'pe':
                    continue
                if is_write:
                    continue
            deps.add(r[4])

    def _record(self, ap, is_write, eng, tok):
        name = ap.tensor.name
        if name not in self.tracked:
            return
        b = self._box(ap)
        lst = self.acc.setdefault(name, [])
        if is_write:
            lst[:] = [r for r in lst if not (b[0] <= r[0] and r[1] <= b[1] and b[2] <= r[2] and r[3] <= b[3])]
            lst.append([b[0], b[1], b[2], b[3], tok, True, eng])
        else:
            for r in lst:
                if (not r[5]) and r[6] == eng and b[0] <= r[0] and r[1] <= b[1] and b[2] <= r[2] and r[3] <= b[3]:
                    r[0], r[1], r[2], r[3], r[4] = b[0], b[1], b[2], b[3], tok
                    return
            lst.append([b[0], b[1], b[2], b[3], tok, False, eng])

    def op(self, eng, fn, reads=(), writes=(), signal=True):
        deps = set()
        for ap in reads:
            self._deps(ap, False, eng, deps)
        for ap in writes:
            self._deps(ap, True, eng, deps)
        if eng == 'sp':
            j = self.dma_n % NDS
            self.dma_n += 1
            if self.dma_cnt[j] > 0:
                deps.add(('d%d' % j, self.dma_cnt[j]))
            self.dma_cnt[j] += 1
            tok = ('d%d' % j, self.dma_cnt[j])
            signal = True
        else:
            tok = (eng, self.cnt[eng] + 1)
            if signal:
                self.cnt[eng] += 1
        kn = self.known[eng]
        need = {}
        for te, v in deps:
            if kn.get(te, 0) >= v:
                continue
            if v > need.get(te, 0):
                need[te] = v
        waits = []
        for te, v in need.items():
            if kn.get(te, 0) >= v:
                continue
            waits.append((te, v))
            sn = self.snap.get((te, v))
            if sn:
                for k2, v2 in sn.items():
                    if kn.get(k2, 0) < v2:
                        kn[k2] = v2
            kn[te] = v
        self.streams[eng].append((waits, fn, tok if signal else None))
        if signal:
            self.snap[tok] = dict(kn)
        for ap in reads:
            self._record(ap, False, eng, tok)
        for ap in writes:
            self._record(ap, True, eng, tok)
        return tok

    def finish(self):
        waits = [('d%d' % j, self.dma_cnt[j]) for j in range(NDS) if self.dma_cnt[j] > 0]
        self.streams['sp'].append((waits, None, None))

    def emit(self, nc, stack):
        sems = {}

        def sem_of(te, v):
            if te[0] == 'd' and te[1:].isdigit():
                key = te
                val = 16 * v
            else:
                ep = (v - 1) // EP
                key = '%s_%d' % (te, ep)
                val = v - ep * EP
            if key not in sems:
                sems[key] = stack.enter_context(nc.semaphore('s_' + key))
            return sems[key], val

        for eng, st in self.streams.items():
            for waits, fn, tok in st:
                for te, v in waits:
                    sem_of(te, v)
                if tok:
                    sem_of(*tok)
        block = stack.enter_context(nc.Block())
        streams = self.streams

        def run(eng, e):
            for waits, fn, tok in streams[eng]:
                for te, v in waits:
                    s, val = sem_of(te, v)
                    e.wait_ge(s, val)
                if fn is None:
                    continue
                inst = fn(e)
                if tok:
                    s, _ = sem_of(*tok)
                    inst.then_inc(s, 16 if eng == 'sp' else 1)

        @block.tensor
        def _(e):
            run('pe', e)

        @block.scalar
        def _(e):
            run('act', e)

        @block.vector
        def _(e):
            run('dve', e)

        @block.gpsimd
        def _(e):
            run('pool', e)

        @block.sync
        def _(e):
            run('sp', e)
        return sems


class Ops:
    def __init__(self, S):
        self.S = S

    def mm(self, out, lhsT, rhs, start=True, stop=True, sig=True):
        self.S.op('pe', lambda e: e.matmul(out, lhsT, rhs, start=start, stop=stop),
                  reads=[lhsT, rhs], writes=[out], signal=sig)

    def tr(self, out, in_, ident, sig=True):
        self.S.op('pe', lambda e: e.transpose(out, in_, ident), reads=[in_, ident], writes=[out], signal=sig)

    def act(self, out, in_, func, bias=None, scale=None, accum=None, eng='act'):
        reads = [in_]
        kw = {}
        if bias is not None:
            kw['bias'] = bias
            if not isinstance(bias, (int, float)):
                reads.append(bias)
        if scale is not None:
            kw['scale'] = scale
            if not isinstance(scale, (int, float)):
                reads.append(scale)
        writes = [out]
        if accum is not None:
            kw['accum_out'] = accum
            writes.append(accum)
        self.S.op('act', lambda e: e.activation(out, in_, func, **kw), reads=reads, writes=writes)

    def tt(self, eng, out, a, b, op):
        self.S.op(eng, lambda e: e.tensor_tensor(out, a, b, op), reads=[a, b], writes=[out])

    def ts(self, eng, out, a, s1, op0, s2=None, op1=None):
        reads = [a]
        if not isinstance(s1, (int, float)):
            reads.append(s1)
        if s2 is not None and not isinstance(s2, (int, float)):
            reads.append(s2)
        if op1 is None:
            self.S.op(eng, lambda e: e.tensor_scalar(out, a, s1, None, op0), reads=reads, writes=[out])
        else:
            self.S.op(eng, lambda e: e.tensor_scalar(out, a, s1, s2, op0, op1), reads=reads, writes=[out])

    def stt(self, eng, out, a, s, b, op0, op1):
        reads = [a, b]
        if not isinstance(s, (int, float)):
            reads.append(s)
        self.S.op(eng, lambda e: e.scalar_tensor_tensor(out, a, s, b, op0, op1), reads=reads, writes=[out])

    def cp(self, eng, out, a):
        if eng == 'act':
            self.S.op('act', lambda e: e.copy(out, a), reads=[a], writes=[out])
        else:
            self.S.op(eng, lambda e: e.tensor_copy(out, a), reads=[a], writes=[out])

    def memset(self, eng, out, v):
        self.S.op(eng, lambda e: e.memset(out, v), reads=[], writes=[out])

    def scan(self, out, d0, d1, init, op0, op1):
        self.S.op('dve', lambda e: e.tensor_tensor_scan(out, d0, d1, init, op0, op1), reads=[d0, d1], writes=[out])

    def red(self, out, a, op=ALU.add, axis=AX.X):
        self.S.op('dve', lambda e: e.tensor_reduce(out, a, axis, op), reads=[a], writes=[out])

    def recip(self, out, a):
        self.S.op('dve', lambda e: e.reciprocal(out, a), reads=[a], writes=[out])

    def dma(self, out, in_):
        self.S.op('sp', lambda e: e.dma_start(out=out, in_=in_), reads=[in_], writes=[out])

from concourse.bass_utils import run_bass_kernel_spmd

D_IN = 4744
CC = 0.5 * float(np.exp(-0.5))
EPS = 1e-6
GN_EPS = 64e-5
LN_EPS = 1e-6

_pc_names = [('MU', 13), ('W0', 4), ('A0', 4), ('KK', 4), ('KA', 4), ('RK', 4), ('GNW', 4), ('GNB', 4),
             ('CW', 32), ('CB', 8), ('LNW', 4), ('SKIP', 4), ('GAIN', 8), ('BSH', 8), ('BSC', 8), ('C', 8),
             ('BI', 1), ('BF', 1)]
_pc_der = [('OMM', 13), ('HW0', 4), ('HA0', 4), ('HKK', 4), ('NKK', 4), ('HKA', 4), ('OMHKA', 4), ('HCW', 32),
           ('HCB', 8), ('HLNW', 4), ('HBF', 1), ('G', 8), ('SH', 8)]
PCI = {}
_o = 0
for _n, _w in _pc_names:
    PCI[_n] = _o
    _o += _w
NPC_IN = _o
for _n, _w in _pc_der:
    PCI[_n] = _o
    _o += _w
NPC = _o


def host_consts():
    c = np.zeros((128, 1152), np.float32)
    p = np.arange(128)[:, None]
    f = np.arange(128)[None, :]
    c[:, 0:128] = (p == f)
    c[:, 128:256] = (f > p)
    c[:, 256:384] = (f >= p)
    c[:, 384:512] = (f < p)
    c[:, 512:640] = (p // 64 == f // 64)
    for h in range(4):
        c[h, 640 + h * 128:640 + (h + 1) * 128] = 1.0
    return c


def host_consts2():
    c = np.zeros((128, 896), np.float32)
    sidx = np.arange(128)[:, None]
    tidx = np.arange(128)[None, :]
    for k in range(7):
        b = 1 << k
        c[:, k * 128:(k + 1) * 128] = ((tidx // (2 * b) == sidx // (2 * b)) & (tidx % (2 * b) >= b) & (sidx % (2 * b) < b))
    return c


def host_pcol(inp, b):
    pc = np.zeros((128, NPC_IN), np.float32)

    def put(name, vec):
        v = np.asarray(vec, np.float32).reshape(-1, 128).T
        pc[:, PCI[name]:PCI[name] + v.shape[1]] = v
    put('MU', inp['mu_rwkv'][0])
    put('W0', inp['w_decay0'][0]); put('A0', inp['a0'][0]); put('KK', inp['k_k'][0]); put('KA', inp['k_a'][0])
    put('RK', inp['r_k'][0]); put('GNW', inp['rwkv_gn_w'][0]); put('GNB', inp['rwkv_gn_b'][0])
    put('CW', inp['mlstm_conv_w'][0].reshape(-1)); put('CB', inp['mlstm_conv_b'][0])
    put('LNW', inp['mlstm_ln_w'][0]); put('SKIP', inp['mlstm_skip'][0]); put('GAIN', inp['norm_gain'][0])
    put('BSH', inp['b_ada'][0][0:1024]); put('BSC', inp['b_ada'][0][1024:2048]); put('C', inp['c'][b])
    pc[0:4, PCI['BI']] = inp['mlstm_b_i'][0]
    pc[0:4, PCI['BF']] = inp['mlstm_b_f'][0]
    return pc


def build(NCH=32):
    nc = bass.Bass("TRN2", target_bir_lowering=False)
    T = NCH * 128
    x = nc.dram_tensor("x", [T, 1024], F32, kind="ExternalInput").ap()
    w_in = nc.dram_tensor("w_in", [1024, D_IN], F32, kind="ExternalInput").ap()
    w_out = nc.dram_tensor("w_out", [1024, 1024], F32, kind="ExternalInput").ap()
    w_ada = nc.dram_tensor("w_ada", [1024, 3072], F32, kind="ExternalInput").ap()
    lora = nc.dram_tensor("lora", [128, 512], F32, kind="ExternalInput").ap()
    pcol = nc.dram_tensor("pcol", [128, NPC_IN], F32, kind="ExternalInput").ap()
    cst = nc.dram_tensor("cst", [128, 1152], F32, kind="ExternalInput").ap()
    cst2 = nc.dram_tensor("cst2", [128, 896], F32, kind="ExternalInput").ap()
    bgate = nc.dram_tensor("bgate", [1, 1024], F32, kind="ExternalInput").ap()
    fgain = nc.dram_tensor("fgain", [1, 1024], F32, kind="ExternalInput").ap()
    y = nc.dram_tensor("y", [T, 1024], F32, kind="ExternalOutput").ap()
    import os as _os
    DBG = bool(_os.environ.get("DBG"))
    if DBG:
        dbg = nc.dram_tensor("dbg", [NCH, 128, 4096], F32, kind="ExternalOutput").ap()
    S = Sched()
    O = Ops(S)
    P = PCI
    with contextlib.ExitStack() as st:
        def sb(name, shape, dt=F32):
            return S.track(st.enter_context(nc.sbuf_tensor(name, shape, dt)))

        def ps(name, shape, dt=F32):
            return S.track(st.enter_context(nc.psum_tensor(name, shape, dt)))
        pb = [ps("pb%d" % i, [128, 512]) for i in range(6)]
        pt = [ps("pt%d" % i, [128, 1024], BF16) for i in range(2)]
        rot = [0]

        def nb():
            b = pb[rot[0] % 5]
            rot[0] += 1
            return b
        Wb = sb("Wb", [128, 8, D_IN], BF16)
        Wo = sb("Wo", [128, 8, 1024], BF16)
        loraB = sb("loraB", [128, 512], BF16)
        pc = sb("pc", [128, NPC])
        cB = sb("cB", [128, 2048], BF16)
        identB = cB[:, 0:128]; ms = cB[:, 128:256]; mTs = cB[:, 256:384]; mTi = cB[:, 384:512]
        bonesB = cB[:, 512:640]; selB = cB[:, 640:1152]
        mlev = [cB[:, 1152 + k * 128:1152 + (k + 1) * 128] for k in range(7)]

        def rep4(m2d):
            return m2d.unsqueeze(1).broadcast_to([128, 4, 128])

        def N4(t2d):
            return t2d.rearrange("p (h c) -> p h c", h=4)
        tmpg = sb("tmpg", [128, 1, 256]); fg_b3 = sb("fg_b", [128, 1, 1024])
        fg_b = fg_b3[:, 0, :]
        onesF = sb("onesF", [128, 128]); mhalf = sb("mhalf", [128, 8]); mone = sb("mone", [128, 8])
        arena = sb("arena", [128, 4096])
        xb = arena[:, 0:1024]; hres = arena[:, 1024:2048]; outb = arena[:, 2048:3072]; misc = arena[:, 3072:4096]
        ysq = arena[:, 3072:3584]; nsq = arena[:, 3584:4096]
        stg = [arena[:, 0:2048].rearrange("p (k n) -> p k n", k=8), arena[:, 2048:4096].rearrange("p (k n) -> p k n", k=8)]
        xsb = sb("xsb", [128, 1024], BF16)
        hnT = sb("hnT", [128, 8, 128], BF16)
        ssq = sb("ssq", [128, 8])
        A = sb("A", [128, 13, 128], BF16); cf = arena[:, 1024:2176]; Bt = sb("Bt", [128, 1, 129]); halo = sb("halo", [128, 13])
        lor = sb("lor", [128, 128], BF16)
        th = sb("th", [128, 1, 128]); tha = sb("tha", [128, 1, 128]); cum = sb("cum", [128, 1, 128])
        ET = sb("ET", [128, 1, 129]); Einv = sb("Einv", [128, 1, 128])
        sq = sb("sq", [128, 1, 128], BF16); rs = sb("rs", [128, 1, 128]); f1 = sb("f1", [128, 1, 128])
        kmod = sb("kmod", [128, 1, 128]); t1 = sb("t1", [128, 1, 128]); rk = sb("rk", [128, 1, 128], BF16)
        aT = [sb("aT%d" % i, [128, 8, 128], BF16) for i in range(2)]; bT = [sb("bT%d" % i, [128, 8, 128], BF16) for i in range(2)]
        kT = [sb("kT%d" % i, [128, 8, 128], BF16) for i in range(2)]; rT = [sb("rT%d" % i, [128, 8, 128], BF16) for i in range(2)]; bhT = sb("bhT", [128, 4, 128], BF16); khT = sb("khT", [128, 4, 128], BF16)
        vbf = sb("vbf", [128, 4, 128], BF16); bonus = [sb("bonus%d" % i, [128, 4, 128], BF16) for i in range(2)]; szr = [sb("szr%d" % i, [128, 4, 128], BF16) for i in range(2)]
        zh = sb("zh", [128, 1, 128]); thz = sb("thz", [128, 1, 128])
        WLs = [sb("WLs%d" % i, [128, 4]) for i in range(2)]
        btok = sb("btok", [128, 512], BF16); ktok = sb("ktok", [128, 512], BF16); vtok = sb("vtok", [128, 512], BF16)
        sAk = [sb("sAk%d" % i, [128, 512], BF16) for i in range(2)]; sRb = [sb("sRb%d" % i, [128, 512], BF16) for i in range(2)]; sRk = [sb("sRk%d" % i, [128, 512], BF16) for i in range(2)]
        Nb = [sb("Nb%d" % i, [128, 512], BF16) for i in range(2)]; Mbb = [sb("Mbb%d" % i, [128, 512], BF16) for i in range(2)]
        Gs = [sb("Gs%d" % i, [128, 512], BF16) for i in range(2)]
        MTb = [sb("MTb%d" % i, [128, 512], BF16) for i in range(2)]
        Xs = sb("Xs", [128, 512], BF16); SAs = sb("SAs", [128, 512], BF16)
        Sst = sb("Sst", [128, 4, 64]); Sbf = sb("Sbf", [128, 4, 64], BF16); Sd = sb("Sd", [128, 4, 64])
        gs = sb("gs", [128, 48]); yn = sb("yn", [128, 512], BF16); scm = yn; hnb = yn; y1 = sb("y1", [128, 1, 128])
        Rq = sb("Rq", [128, 8, 131], BF16); acc = sb("acc", [128, 2, 128]); thq = sb("thq", [128, 512], BF16)
        qc = [sb("qc%d" % i, [128, 4, 128], BF16) for i in range(2)]; loraF = arena[:, 2176:2688]; kc = sb("kc", [128, 4, 128], BF16)
        qt = [sb("qt%d" % i, [128, 4, 128], BF16) for i in range(2)]; kt_ = [sb("kt_%d" % i, [128, 4, 128], BF16) for i in range(2)]
        A1 = [sb("A1_%d" % i, [128, 4, 128], BF16) for i in range(2)]; szm = [sb("szm%d" % i, [128, 4, 128], BF16) for i in range(2)]; tho = sb("tho", [128, 1, 128])
        gsm = sb("gsm", [4, 5, 128]); gsb = sb("gsb", [4, 4, 128], BF16)
        e1L = [sb("e1L%d" % i, [128, 4]) for i in range(2)]
        vmtok = [sb("vmtok%d" % i, [128, 4, 129], BF16) for i in range(2)]; kmtok = sb("kmtok", [128, 512], BF16)
        Cst = sb("Cst", [128, 4, 129]); Cbf = sb("Cbf", [128, 4, 129], BF16); Cd = sb("Cd", [128, 2, 129])
        mst = sb("mst", [128, 12, 4]); y2 = sb("y2", [128, 1, 128])
        yT = sb("yT", [128, 8, 128], BF16)
        cact = sb("cact", [128, 24]); cactB = sb("cactB", [128, 8], BF16); cbB = Wo[:, :, 512:640]
        wab = [Wo[:, :, i * 256:(i + 1) * 256] for i in range(2)]

        def pcs(name, i=0, rows=slice(0, 128)):
            return pc[rows, P[name] + i:P[name] + i + 1]

        O.dma(pc[:, 0:NPC_IN], pcol)
        O.dma(cf[:], cst)
        O.dma(loraF[:], lora)
        O.dma(fg_b3[:], fgain.partition_broadcast(128))
        O.memset('dve', onesF[:], 1.0); O.memset('dve', mhalf[:], -0.5); O.memset('dve', mone[:], -1.0)
        O.memset('dve', halo[:], 0.0); O.memset('dve', Rq[:], 0.0); O.memset('dve', Sst[:], 0.0)
        O.memset('dve', Sbf[:], 0.0); O.memset('dve', Cst[:], 0.0); O.memset('dve', Cbf[:], 0.0)
        O.memset('dve', ET[:], 1.0); O.memset('dve', vmtok[0][:], 1.0); O.memset('dve', vmtok[1][:], 1.0)
        for z_ in aT + bT + kT + rT:
            O.memset('dve', z_[:], 0.0)
        O.dma(arena[:, 0:896], cst2)
        gate_bf = xsb
        O.cp('dve', identB, cf[:, 0:128])
        O.cp('dve', ms, cf[:, 384:512])
        O.cp('dve', mTs, cf[:, 128:256])
        O.cp('dve', mTi, cf[:, 256:384])
        O.cp('dve', cB[:, 1152:2048], arena[:, 0:896])
        O.cp('dve', bonesB, cf[:, 512:640])
        O.cp('dve', selB, cf[:, 640:1152])
        O.cp('dve', loraB[:], loraF[:])

        def dts(dst, src, n, m, a):
            O.ts('dve', pc[:, P[dst]:P[dst] + n], pc[:, P[src]:P[src] + n], m, ALU.mult, a, ALU.add)
        dts('OMM', 'MU', 13, -1.0, 1.0); dts('HW0', 'W0', 4, 0.5, 0.0); dts('HA0', 'A0', 4, 0.5, 0.0)
        dts('HKK', 'KK', 4, 0.5, 0.0); dts('NKK', 'KK', 4, -1.0, 0.0); dts('HKA', 'KA', 4, 0.5, 0.0)
        dts('OMHKA', 'KA', 4, -0.5, 1.0); dts('HCW', 'CW', 32, 0.5, 0.0); dts('HCB', 'CB', 8, 0.5, 0.0)
        dts('HLNW', 'LNW', 4, 0.5, 0.0); dts('HBF', 'BF', 1, 0.5, 0.0)
        O.act(cact[:, 0:8], pc[:, P['C']:P['C'] + 8], AF.Tanh, scale=0.5)
        O.ts('dve', cact[:, 8:16], pc[:, P['C']:P['C'] + 8], 0.5, ALU.mult)
        O.stt('dve', cact[:, 16:24], cact[:, 0:8], 1.0, cact[:, 8:16], ALU.add, ALU.mult)
        O.cp('dve', cactB[:], cact[:, 16:24])
        for kt in range(8):
            O.cp('dve', cbB[:, kt, :], cact[:, 16 + kt:17 + kt].broadcast_to([128, 128]))
        wada_v = w_ada.rearrange("(kt p) n -> p kt n", p=128)
        pcp = pb[5]
        for j in range(12):
            sg = stg[j % 2]
            wb_ = wab[j % 2]
            O.dma(sg[:, :, 0:256], wada_v[:, :, j * 256:(j + 1) * 256])
            O.cp(('act', 'dve')[j % 2], wb_[:], sg[:, :, 0:256])
            if j < 8:
                for sub in range(2):
                    ft = j * 2 + sub
                    for kt in range(8):
                        O.mm(pcp[:, ft:ft + 1], wb_[:, kt, sub * 128:(sub + 1) * 128], cactB[:, kt:kt + 1],
                             start=(kt == 0), stop=(kt == 7), sig=(kt == 7))
            else:
                pg = nb()
                for kt in range(8):
                    O.mm(pg[:, 0:256], cbB[:, kt, :], wb_[:, kt, :], start=(kt == 0), stop=(kt == 7), sig=(kt == 7))
                O.dma(tmpg[:], bgate[:, (j - 8) * 256:(j - 7) * 256].partition_broadcast(128))
                O.tt('dve', gate_bf[:, (j - 8) * 256:(j - 7) * 256], pg[:, 0:256], tmpg[:, 0, :], ALU.add)
        O.tt('dve', pc[:, P['SH']:P['SH'] + 8], pcp[:, 0:8], pc[:, P['BSH']:P['BSH'] + 8], ALU.add)
        O.tt('dve', cact[:, 0:8], pcp[:, 8:16], pc[:, P['BSC']:P['BSC'] + 8], ALU.add)
        O.stt('dve', pc[:, P['G']:P['G'] + 8], cact[:, 0:8], 1.0, pc[:, P['GAIN']:P['GAIN'] + 8], ALU.add, ALU.mult)
        win_v = w_in.rearrange("(kt p) n -> p kt n", p=128)
        wout_v = w_out.rearrange("(kt p) n -> p kt n", p=128)
        engs = ('act', 'dve', 'pool')
        j = 0
        for c0 in range(0, D_IN, 256):
            w = min(256, D_IN - c0)
            sg = stg[j % 2]
            O.dma(sg[:, :, 0:w], win_v[:, :, c0:c0 + w])
            O.cp(engs[j % 3], Wb[:, :, c0:c0 + w], sg[:, :, 0:w])
            j += 1
        for c0 in range(0, 1024, 256):
            sg = stg[j % 2]
            O.dma(sg[:, :, 0:256], wout_v[:, :, c0:c0 + 256])
            O.cp(engs[j % 3], Wo[:, :, c0:c0 + 256], sg[:, :, 0:256])
            j += 1
        for j2 in range(8):
            O.tt(('dve', 'pool')[j2 % 2], Wo[:, j2, :], Wo[:, j2, :], gate_bf[:], ALU.mult)

        def hs(tile, h):
            return tile[:, h, :]

        tiles = [(128 * i, ('lerp', i)) for i in range(13)] + [(1664 + 128 * i, ('zr', i)) for i in range(4)] \
            + [(2176 + 128 * i, ('mqk', i)) for i in range(4)] + [(2688 + 128 * i, ('mqk', 4 + i)) for i in range(4)] \
            + [(3712 + 128 * i, ('mo', i)) for i in range(4)] + [(4232 + 128 * i, ('zm', i)) for i in range(4)]

        poolM = [0]
        poolS = [0]

        def nbM():
            b_ = pb[poolM[0] % 4]
            poolM[0] += 1
            return b_

        def nbS():
            b_ = pb[4 + poolS[0] % 2]
            poolS[0] += 1
            return b_
        ptM = pt[0]
        ptS = pt[1]

        def gen_PB(c):
            t0 = c * 128
            par = c % 2
            if c == 0:
                O.dma(xb, x[t0:t0 + 128, :])
            O.act(xsb[:], xb, AF.Square, accum=ssq[:, 0:1])
            yield
            O.ts('dve', ssq[:, 1:2], ssq[:, 0:1], 1.0 / 1024, ALU.mult, EPS, ALU.add)
            O.tt('pool', ssq[:, 2:3], ssq[:, 1:2], mhalf[:, 0:1], ALU.pow)
            yield
            O.ts('dve', xsb[:], xb, ssq[:, 2:3], ALU.mult)
            if c + 1 < NCH:
                O.dma(xb, x[t0 + 128:t0 + 256, :])
            yield
            for half in range(2):
                for q in range(4):
                    kt = half * 4 + q
                    O.tr(ptS[:, q * 128:(q + 1) * 128], xsb[:, kt * 128:(kt + 1) * 128], identB, sig=(q == 3))
                yield
                for q in range(4):
                    kt = half * 4 + q
                    O.act(hnT[:, kt, :], ptS[:, q * 128:(q + 1) * 128], AF.Identity,
                          bias=pcs('SH', kt), scale=pcs('G', kt))
                yield

            def handle(hd, Pp):
                kind, i = hd
                if kind == 'lerp':
                    bt = Bt[:, 0, :]
                    O.cp('dve', bt[:, 0:1], halo[:, i:i + 1])
                    O.act(bt[:, 1:129], Pp, AF.Identity, scale=pcs('MU', i))
                    O.stt('dve', A[:, i, :], Pp, pcs('OMM', i), bt[:, 0:128], ALU.mult, ALU.add)
                    O.cp('dve', halo[:, i:i + 1], bt[:, 128:129])
                elif kind in ('zr', 'zm'):
                    dst = szr[par] if kind == 'zr' else szm[par]
                    O.act(zh[:, 0, :], Pp, AF.Identity, scale=0.5)
                    O.act(thz[:, 0, :], Pp, AF.Tanh, scale=0.5)
                    O.ts('pool', thz[:, 0, :], thz[:, 0, :], 1.0, ALU.add, 1.0, ALU.mult)
                    O.tt('pool', dst[:, i, :], thz[:, 0, :], zh[:, 0, :], ALU.mult)
                elif kind == 'mo':
                    O.act(tho[:, 0, :], Pp, AF.Tanh, scale=0.5)
                    O.ts('pool', A1[par][:, i, :], tho[:, 0, :], 1.0, ALU.add, pcs('HLNW', i), ALU.mult)
                elif kind == 'mqk':
                    rq = Rq[:, i, :]
                    O.cp('dve', rq[:, 0:3], rq[:, 128:131])
                    O.cp('act', rq[:, 3:131], Pp)
            GW = 4
            deferred = []
            for gi in range(0, len(tiles), GW):
                bank = nbS()
                grp = tiles[gi:gi + GW]
                for q, (col0, hd) in enumerate(grp):
                    for kt in range(8):
                        O.mm(bank[:, q * 128:(q + 1) * 128], Wb[:, kt, col0:col0 + 128], hnT[:, kt, :],
                             start=(kt == 0), stop=(kt == 7), sig=(kt == 7 and q == len(grp) - 1))
                yield
                ready, deferred = deferred, []
                for fn_ in ready:
                    fn_()
                for q, (col0, hd) in enumerate(grp):
                    handle(hd, bank[:, q * 128:(q + 1) * 128])
                yield
            for fn_ in deferred:
                fn_()
            deferred = []
            yield
            T4 = thq[:].rearrange("p (a b) -> p a b", a=4)
            for gq, dst4 in enumerate((qc[par], kc)):
                D4 = dst4[:]

                def wb(col):
                    return pc[:, col:col + 4].unsqueeze(2).broadcast_to([128, 4, 128])
                O.tt('dve', D4, Rq[:, gq * 4:gq * 4 + 4, 0:128], wb(P['HCW'] + gq * 4), ALU.mult)
                O.tt('dve', D4, D4, wb(P['HCB'] + gq * 4), ALU.add)
                for tap in range(1, 4):
                    O.tt('dve', T4, Rq[:, gq * 4:gq * 4 + 4, tap:tap + 128], wb(P['HCW'] + tap * 8 + gq * 4), ALU.mult)
                    O.tt('dve', D4, D4, T4, ALU.add)
                yield
                d2 = dst4[:].rearrange("p a b -> p (a b)")
                O.act(thq[:], d2, AF.Tanh)
                yield
                O.stt('dve', d2, thq[:], 1.0, d2, ALU.add, ALU.mult)
                yield
            bank = nbS()
            for gg, col0 in enumerate((4224, 4228)):
                for kt in range(8):
                    O.mm(bank[0:4, gg * 128:(gg + 1) * 128], Wb[:, kt, col0:col0 + 4], hnT[:, kt, :],
                         start=(kt == 0), stop=(kt == 7), sig=(kt == 7 and gg == 1))
            yield
            ei, thf, E1, rE1, E2 = [gsm[:, k, :] for k in range(5)]
            sf = thf
            E1h, E1l, E2h, E2l = [gsb[:, k, :] for k in range(4)]
            O.act(ei, bank[0:4, 0:128], AF.Exp, bias=pcs('BI', 0, slice(0, 4)))
            O.act(thf, bank[0:4, 128:256], AF.Tanh, bias=pcs('HBF', 0, slice(0, 4)), scale=0.5)
            yield
            O.ts('dve', sf, thf, 0.5, ALU.mult, 0.5, ALU.add)
            O.scan(E1, sf, onesF[0:4, :], 1.0, ALU.mult, ALU.mult)
            O.recip(rE1, E1)
            O.stt('dve', E2, ei, 128.0 ** -0.5, rE1, ALU.mult, ALU.mult)
            O.cp('dve', E1h, E1); O.tt('dve', E1l, E1, E1h, ALU.subtract)
            O.cp('dve', E2h, E2); O.tt('dve', E2l, E2, E2h, ALU.subtract)
            yield
            bkb = nbS(); bkc = nbS()
            for h in range(4):
                O.mm(bkb[:, h * 128:(h + 1) * 128], selB[0:4, h * 128:(h + 1) * 128], E1h, start=True, stop=False, sig=False)
                O.mm(bkb[:, h * 128:(h + 1) * 128], selB[0:4, h * 128:(h + 1) * 128], E1l, start=False, stop=True, sig=(h == 3))
            for h in range(4):
                O.mm(bkc[:, h * 128:(h + 1) * 128], selB[0:4, h * 128:(h + 1) * 128], E2h, start=True, stop=False, sig=False)
                O.mm(bkc[:, h * 128:(h + 1) * 128], selB[0:4, h * 128:(h + 1) * 128], E2l, start=False, stop=True, sig=(h == 3))
            yield
            for h in range(4):
                O.cp('dve', e1L[par][:, h:h + 1], bkb[:, h * 128 + 127:h * 128 + 128])
                O.tt('dve', qt[par][:, h, :], qc[par][:, h, :], bkb[:, h * 128:(h + 1) * 128], ALU.mult)
                O.tt('dve', kt_[par][:, h, :], kc[:, h, :], bkc[:, h * 128:(h + 1) * 128], ALU.mult)
            yield
            bank = nbS()
            for kt in range(8):
                O.mm(bank[:, 0:512], hnT[:, kt, :], Wb[:, kt, 3200:3712], start=(kt == 0), stop=(kt == 7), sig=(kt == 7))
            yield
            O.cp('act', vmtok[par][:, :, 0:128], bank[:, 0:512].rearrange("p (h d) -> p h d", h=4))
            yield

        def gen_C1(c):
            par = c % 2
            O.act(lor[0:64, :], A[0:64, 12, :], AF.Tanh)
            O.cp('dve', lor[64:128, :], A[64:128, 12, :])
            yield
            for i in range(4):
                sl = slice(i * 128, (i + 1) * 128)
                bkA = nbS(); bkB = nbS()
                bzs = bkA[:, 0:128]; bsss = bkA[:, 128:256]; bbns = bkA[:, 256:384]; bas = bkB[:, 0:128]
                O.mm(bzs, loraB[0:64, sl], lor[0:64, :])
                O.mm(bas, loraB[64:128, sl], lor[64:128, :])
                Ak = A[:, 4 + i, :]; Ar = A[:, i, :]; Av = A[:, 8 + i, :]
                O.act(sq[:, 0, :], Ak, AF.Square, scale=pcs('KK', i))
                O.cp('act', vbf[:, i, :], Av)
                yield
                O.act(th[:, 0, :], bzs, AF.Tanh, bias=pcs('HW0', i), scale=0.5)
                O.act(tha[:, 0, :], bas, AF.Tanh, bias=pcs('HA0', i), scale=0.5)
                O.mm(bsss, bonesB, sq[:, 0, :])
                yield
                O.scan(cum[:, 0, :], th[:, 0, :], onesF[:], 0.0, ALU.add, ALU.add)
                O.ts('pool', f1[:, 0, :], tha[:, 0, :], pcs('HKA', i), ALU.mult, pcs('OMHKA', i), ALU.add)
                O.tt('pool', kmod[:, 0, :], Ak, f1[:, 0, :], ALU.mult)
                O.ts('pool', t1[:, 0, :], tha[:, 0, :], pcs('HKK', i), ALU.mult, pcs('HKK', i), ALU.add)
                O.tt('pool', t1[:, 0, :], t1[:, 0, :], Ak, ALU.mult)
                O.ts('dve', rs[:, 0, :], bsss, 1e-24, ALU.max)
                O.recip(rs[:, 0, :], rs[:, 0, :])
                yield
                O.act(ET[:, 0, 1:129], cum[:, 0, :], AF.Exp, scale=-CC)
                O.act(Einv[:, 0, :], cum[:, 0, :], AF.Exp, scale=CC)
                yield
                O.stt('dve', rk[:, 0, :], Ar, pcs('RK', i), kmod[:, 0, :], ALU.mult, ALU.mult)
                O.tt('dve', t1[:, 0, :], t1[:, 0, :], rs[:, 0, :], ALU.mult)
                O.mm(bbns, bonesB, rk[:, 0, :])
                yield
                WL = ET[:, 0, 128:129]
                O.stt('dve', bhT[:, i, :], t1[:, 0, :], WL, Einv[:, 0, :], ALU.mult, ALU.mult)
                O.stt('dve', khT[:, i, :], kmod[:, 0, :], WL, Einv[:, 0, :], ALU.mult, ALU.mult)
                for p_ in range(2):
                    rw = slice(p_ * 64, p_ * 64 + 64)
                    hh = 2 * i + p_
                    O.tt('dve', bT[par][rw, hh, :], t1[rw, 0, :], Einv[rw, 0, :], ALU.mult)
                    O.stt('dve', aT[par][rw, hh, :], A[rw, 4 + i, :], pc[rw, P['NKK'] + i:P['NKK'] + i + 1], ET[rw, 0, 0:128],
                          ALU.mult, ALU.mult)
                    O.tt('pool', kT[par][rw, hh, :], kmod[rw, 0, :], Einv[rw, 0, :], ALU.mult)
                    O.tt('pool', rT[par][rw, hh, :], A[rw, i, :], ET[rw, 0, 1:129], ALU.mult)
                O.tt('dve', bonus[par][:, i, :], bbns, Av, ALU.mult)
                O.cp('dve', WLs[par][:, i:i + 1], WL)
                yield

        def gen_C2(c):
            par = c % 2
            O.dma(hres, x[c * 128:(c + 1) * 128, :])
            for i in range(4):
                sl = slice(i * 128, (i + 1) * 128)
                O.tr(ptM[:, sl], bhT[:, i, :], identB, sig=False)
                O.tr(ptM[:, 512 + i * 128:512 + (i + 1) * 128], khT[:, i, :], identB, sig=(i == 3))
            yield
            O.cp('act', btok[:], ptM[:, 0:512]); O.cp('act', ktok[:], ptM[:, 512:1024])
            yield
            for i in range(4):
                sl = slice(i * 128, (i + 1) * 128)
                O.tr(ptM[:, sl], vbf[:, i, :], identB, sig=False)
                O.tr(ptM[:, 512 + i * 128:512 + (i + 1) * 128], kt_[par][:, i, :], identB, sig=(i == 3))
            yield
            O.cp('act', vtok[:], ptM[:, 0:512]); O.cp('act', kmtok[:], ptM[:, 512:1024])
            yield

        def gen_D(c):
            par = c % 2
            G2 = (0, 1)
            HD = [[4 * g + q for q in range(4)] for g in G2]

            def prod(lhs, rhs):
                bks = [nbM(), nbM()]
                for g in G2:
                    for q, h in enumerate(HD[g]):
                        sl = slice(q * 128, (q + 1) * 128)
                        O.mm(bks[g][:, sl], hs(lhs[par], h), hs(rhs[par], h), sig=(q == 3))
                return bks
            bN = prod(aT, bT)
            bNT = prod(bT, aT)
            yield
            for g in G2:
                O.tt('dve', N4(Nb[g][:]), N4(bN[g][:]), rep4(ms), ALU.mult)
                O.tt('dve', N4(MTb[g][:]), N4(bNT[g][:]), rep4(mlev[0]), ALU.mult)
            yield
            bAk = prod(kT, aT)
            bRb = prod(bT, rT)
            for g in G2:
                O.tt('dve', N4(MTb[g][:]), N4(MTb[g][:]), rep4(identB), ALU.add)
            yield
            for g in G2:
                O.cp('act', sAk[g][:], bAk[g][:])
                O.cp('act', sRb[g][:], bRb[g][:])
            yield
            for g in G2:
                O.tt('pool', N4(sAk[g][:]), N4(sAk[g][:]), rep4(mTs), ALU.mult)
                O.tt('pool', N4(sRb[g][:]), N4(sRb[g][:]), rep4(mTi), ALU.mult)
            bRk = prod(kT, rT)
            yield
            for g in G2:
                O.cp('act', sRk[g][:], bRk[g][:])
            yield
            for g in G2:
                O.tt('pool', N4(sRk[g][:]), N4(sRk[g][:]), rep4(mTi), ALU.mult)
            for lv in range(1, 7):
                b2 = [nbM(), nbM()]
                for g in G2:
                    for q in range(4):
                        sl = slice(q * 128, (q + 1) * 128)
                        O.tr(ptM[:, g * 512 + q * 128:g * 512 + (q + 1) * 128], MTb[g][:, sl], identB, sig=(q == 3))
                    for q in range(4):
                        sl = slice(q * 128, (q + 1) * 128)
                        O.mm(b2[g][:, sl], Nb[g][:, sl], MTb[g][:, sl], sig=(q == 3))
                yield
                O.cp('act', Mbb[0][:], ptM[:, 0:512])
                O.cp('act', Mbb[1][:], ptM[:, 512:1024])
                for g in G2:
                    O.tt('dve', N4(Gs[g][:]), N4(b2[g][:]), rep4(mlev[lv]), ALU.mult)
                yield
                b3 = [nbM(), nbM()]
                for g in G2:
                    for q in range(4):
                        sl = slice(q * 128, (q + 1) * 128)
                        O.mm(b3[g][:, sl], identB, MTb[g][:, sl], start=True, stop=False, sig=False)
                        O.mm(b3[g][:, sl], Mbb[g][:, sl], Gs[g][:, sl], start=False, stop=True, sig=(q == 3))
                yield
                O.cp('act', MTb[0][:], b3[0][:])
                O.cp('dve', MTb[1][:], b3[1][:])
                yield
            bX = nbM()
            for g in G2:
                for q, h in enumerate(HD[g]):
                    O.mm(bX[:, h * 64:(h + 1) * 64], hs(aT[par], h), Sbf[:, h // 2, :], start=True, stop=False, sig=False)
                    O.mm(bX[:, h * 64:(h + 1) * 64], sAk[g][:, q * 128:(q + 1) * 128], vtok[:, h * 64:(h + 1) * 64],
                         start=False, stop=True, sig=(h == 7))
            yield
            O.cp('act', Xs[:], bX[:, 0:512])
            yield
            bS = nbM()
            for g in G2:
                for q, h in enumerate(HD[g]):
                    O.mm(bS[:, h * 64:(h + 1) * 64], MTb[g][:, q * 128:(q + 1) * 128], Xs[:, h * 64:(h + 1) * 64], sig=(h == 7))
            yield
            O.cp('act', SAs[:], bS[:, 0:512])
            yield
            Yb = nbM()
            for g in G2:
                for q, h in enumerate(HD[g]):
                    yo = Yb[:, h * 64:(h + 1) * 64]
                    O.mm(yo, hs(rT[par], h), Sbf[:, h // 2, :], start=True, stop=False, sig=False)
                    O.mm(yo, sRb[g][:, q * 128:(q + 1) * 128], SAs[:, h * 64:(h + 1) * 64], start=False, stop=False, sig=False)
                    O.mm(yo, sRk[g][:, q * 128:(q + 1) * 128], vtok[:, h * 64:(h + 1) * 64], start=False, stop=True, sig=(h == 7))
            bSt = nbM()
            for i in range(4):
                sl = slice(i * 128, (i + 1) * 128)
                O.mm(bSt[:, sl], btok[:, sl], SAs[:, sl], start=True, stop=False, sig=False)
                O.mm(bSt[:, sl], ktok[:, sl], vtok[:, sl], start=False, stop=True, sig=(i == 3))
            O.tt('dve', Sd[:], Sst[:], WLs[par][:].unsqueeze(2).broadcast_to([128, 4, 64]), ALU.mult)
            yield
            Yv = Yb[:, 0:512].rearrange("p (h c) -> p h c", h=8)
            O.red(gs[:, 0:8], Yv)
            O.act(ysq, Yb[:, 0:512], AF.Square)
            for p_ in range(2):
                rows = slice(p_ * 64, p_ * 64 + 64)
                O.tt('dve', Sst[rows, :, :], N4(bSt[rows, 0:512])[:, :, p_ * 64:p_ * 64 + 64], Sd[rows, :, :], ALU.add)
            O.cp('act', Sbf[:], Sst[:])
            yield
            O.red(gs[:, 8:16], ysq.rearrange("p (h c) -> p h c", h=8))
            O.ts('dve', gs[:, 16:24], gs[:, 0:8], 1.0 / 64, ALU.mult)
            O.tt('dve', gs[:, 24:32], gs[:, 16:24], gs[:, 16:24], ALU.mult)
            O.stt('dve', gs[:, 32:40], gs[:, 8:16], 1.0 / 64, gs[:, 24:32], ALU.mult, ALU.subtract)
            O.ts('dve', gs[:, 32:40], gs[:, 32:40], GN_EPS, ALU.add)
            O.tt('pool', gs[:, 40:48], gs[:, 32:40], mhalf[:, 0:8], ALU.pow)
            yield
            yield
            ysq3 = ysq.rearrange("p (h c) -> p h c", h=8)
            O.tt('dve', ysq3, Yv, gs[:, 16:24].unsqueeze(2).broadcast_to([128, 8, 64]), ALU.subtract)
            O.tt('dve', yn[:].rearrange("p (h c) -> p h c", h=8), ysq3,
                 gs[:, 40:48].unsqueeze(2).broadcast_to([128, 8, 64]), ALU.mult)
            yield
            for i in range(4):
                O.tr(ptM[:, i * 128:(i + 1) * 128], yn[:, i * 128:(i + 1) * 128], identB, sig=(i == 3))
            yield
            def pcb(name):
                return pc[:, P[name]:P[name] + 4].unsqueeze(2).broadcast_to([128, 4, 128])
            yw = ysq.rearrange("p (h c) -> p h c", h=4)
            O.tt('dve', yw, N4(ptM[:, 0:512]), pcb('GNW'), ALU.mult)
            O.tt('dve', yw, yw, bonus[par][:], ALU.add)
            O.tt('dve', yw, yw, pcb('GNB'), ALU.add)
            O.tt('dve', yT[:, 0:4, :], yw, szr[par][:], ALU.mult)
            yield

        def gen_E(c):
            par = c % 2
            bsc = nbM()
            for h in range(4):
                sl = slice(h * 128, (h + 1) * 128)
                O.mm(bsc[:, sl], kt_[par][:, h, :], qt[par][:, h, :], sig=(h == 3))
            yield
            O.tt('dve', N4(scm[:]), N4(bsc[:]), rep4(mTi), ALU.mult)
            yield
            bH = [nbM(), nbM()]
            bU = [nbM(), nbM()]
            for h in range(4):
                o_ = (h % 2) * 129
                sl = slice(h * 128, (h + 1) * 128)
                O.mm(bH[h // 2][:, o_:o_ + 129], scm[:, sl], vmtok[par][:, h, :], start=True, stop=False, sig=False)
                O.mm(bH[h // 2][:, o_:o_ + 129], qt[par][:, h, :], Cbf[:, h, :], start=False, stop=True, sig=(h % 2 == 1))
            for h in range(4):
                o_ = (h % 2) * 129
                sl = slice(h * 128, (h + 1) * 128)
                O.mm(bU[h // 2][:, o_:o_ + 129], kmtok[:, sl], vmtok[par][:, h, :], sig=(h % 2 == 1))
            yield
            for b2 in range(2):
                Hv = bH[b2][:, 0:258].rearrange("p (h c) -> p h c", h=2)
                s2 = slice(2 * b2, 2 * b2 + 2)
                O.cp('dve', mst[:, 0, s2], Hv[:, :, 128])
                O.red(mst[:, 3, s2], Hv[:, :, 0:128])
                O.act(nsq.rearrange("p (h c) -> p h c", h=4)[:, s2, :], Hv[:, :, 0:128], AF.Square)
            yield
            for b2 in range(2):
                s2 = slice(2 * b2, 2 * b2 + 2)
                O.tt('dve', Cst[:, s2, :], bU[b2][:, 0:258].rearrange("p (h c) -> p h c", h=2), Cst[:, s2, :], ALU.add)
                O.tt('dve', Cst[:, s2, :], Cst[:, s2, :], e1L[par][:, s2].unsqueeze(2).broadcast_to([128, 2, 129]), ALU.mult)
            O.cp('act', Cbf[:], Cst[:])
            yield
            O.stt('dve', mst[:, 1, :], mst[:, 0, :], -1.0, mst[:, 0, :], ALU.mult, ALU.max)
            O.ts('dve', mst[:, 1, :], mst[:, 1, :], 1.0, ALU.max)
            O.recip(mst[:, 2, :], mst[:, 1, :])
            O.red(mst[:, 4, :], nsq.rearrange("p (h c) -> p h c", h=4))
            O.ts('dve', mst[:, 5, :], mst[:, 3, :], 1.0 / 128, ALU.mult)
            O.tt('dve', mst[:, 6, :], mst[:, 5, :], mst[:, 5, :], ALU.mult)
            O.stt('dve', mst[:, 7, :], mst[:, 4, :], 1.0 / 128, mst[:, 6, :], ALU.mult, ALU.subtract)
            O.tt('dve', mst[:, 8, :], mst[:, 2, :], mst[:, 2, :], ALU.mult)
            O.tt('dve', mst[:, 9, :], mst[:, 7, :], mst[:, 8, :], ALU.mult)
            O.ts('dve', mst[:, 9, :], mst[:, 9, :], LN_EPS, ALU.add)
            O.tt('pool', mst[:, 10, :], mst[:, 9, :], mhalf[:, 0:4], ALU.pow)
            yield
            O.tt('dve', mst[:, 11, :], mst[:, 2, :], mst[:, 10, :], ALU.mult)
            O.stt('dve', mst[:, 6, :], mst[:, 5, :], -1.0, mst[:, 11, :], ALU.mult, ALU.mult)
            yield
            for h in range(4):
                o_ = (h % 2) * 129
                O.act(hnb[:, h * 128:(h + 1) * 128], bH[h // 2][:, o_:o_ + 128], AF.Identity,
                      bias=mst[:, 6, h:h + 1], scale=mst[:, 11, h:h + 1])
            yield
            for h in range(4):
                O.tr(ptM[:, h * 128:(h + 1) * 128], hnb[:, h * 128:(h + 1) * 128], identB, sig=(h == 3))
            yield
            nw = nsq.rearrange("p (h c) -> p h c", h=4)
            yw2 = ysq.rearrange("p (h c) -> p h c", h=4)
            O.tt('dve', nw, N4(ptM[:, 0:512]), A1[par][:], ALU.mult)
            O.tt('dve', yw2, qc[par][:], pc[:, P['SKIP']:P['SKIP'] + 4].unsqueeze(2).broadcast_to([128, 4, 128]), ALU.mult)
            O.tt('dve', nw, nw, yw2, ALU.add)
            O.tt('dve', yT[:, 4:8, :], nw, szm[par][:], ALU.mult)
            yield

        def gen_F(c):
            t0 = c * 128
            for half in range(2):
                bo = nbM()
                hsl = slice(half * 512, (half + 1) * 512)
                for j2 in range(8):
                    O.mm(bo[:, 0:512], yT[:, j2, :], Wo[:, j2, hsl], start=(j2 == 0), stop=(j2 == 7), sig=(j2 == 7))
                yield
                O.tt('dve', hres[:, hsl], bo[:, 0:512], hres[:, hsl], ALU.add)
                yield
            O.act(misc, hres, AF.Square, accum=ssq[:, 4:5])
            yield
            O.ts('dve', ssq[:, 5:6], ssq[:, 4:5], 1.0 / 1024, ALU.mult, EPS, ALU.add)
            O.tt('pool', ssq[:, 6:7], ssq[:, 5:6], mhalf[:, 0:1], ALU.pow)
            yield
            O.stt('dve', outb, hres, ssq[:, 6:7], fg_b, ALU.mult, ALU.mult)
            O.dma(y[t0:t0 + 128, :], outb)
            if DBG:
                O.cp('dve', misc[:, 0:1024], yT[:].rearrange("p a b -> p (a b)"))
                O.dma(dbg[c, :, 0:1024], misc[:, 0:1024])
            yield

        def run_merged(items, counts=None):
            items = [[g, float(r), 0.0, 0, k] for k, (g, r) in enumerate(items) if g is not None]
            done = {}
            while items:
                for it in list(items):
                    it[2] += it[1]
                    while it[2] >= 1.0 and it in items:
                        it[2] -= 1.0
                        try:
                            next(it[0])
                            it[3] += 1
                        except StopIteration:
                            items.remove(it)
                            done[it[4]] = it[3]
            return done

        def chain(*gens):
            for g in gens:
                yield from g

        if not _os.environ.get('SKIPLOOP'):
            run_merged([(chain(gen_PB(0), gen_C1(0)), 1)])
            rates = (1.0, 1.0)
            for c in range(NCH):
                main = chain(gen_C2(c), gen_D(c), gen_E(c), gen_F(c))
                side = chain(gen_PB(c + 1), gen_C1(c + 1)) if c + 1 < NCH else None
                done = run_merged([(main, rates[0]), (side, rates[1])])
                if c == 0 and len(done) == 2:
                    n_main, n_side = done[0], done[1]
                    m = float(max(n_main, n_side))
                    rates = (n_main / m, n_side / m)
                    if _os.environ.get('DBGSTEPS'):
                        print('steps main/side', n_main, n_side, rates)
        S.finish()
        S.emit(nc, st)
    return nc


_NC_CACHE = {}


def kernel(**inputs):
    inp = {k: np.asarray(v) for k, v in inputs.items()}
    B, T, D = inp['x'].shape
    NCH = T // 128
    if NCH not in _NC_CACHE:
        _NC_CACHE[NCH] = build(NCH)
    nc = _NC_CACHE[NCH]
    cstv = host_consts()
    lora = np.ascontiguousarray(np.concatenate([inp['w_decay_up'][0], inp['w_icl_up'][0]], axis=0), np.float32)
    shared = {
        "w_in": np.ascontiguousarray(inp['w_in'][0], np.float32),
        "w_out": np.ascontiguousarray(inp['w_out'][0], np.float32),
        "w_ada": np.ascontiguousarray(inp['w_ada'][0], np.float32),
        "lora": lora, "cst": cstv, "cst2": host_consts2(),
        "bgate": np.ascontiguousarray(inp['b_ada'][0][2048:3072].reshape(1, 1024), np.float32),
        "fgain": np.ascontiguousarray(inp['final_gain'].reshape(1, 1024), np.float32),
    }
    in_maps = []
    for b in range(B):
        m = dict(shared)
        m["x"] = np.ascontiguousarray(inp['x'][b], np.float32)
        m["pcol"] = host_pcol(inp, b)
        in_maps.append(m)
    res = run_bass_kernel_spmd(nc, in_maps, core_ids=list(range(B)))
    global LAST_RES
    LAST_RES = res
    return np.stack([np.asarray(r["y"], np.float32) for r in res.results], axis=0)
```

```python
import contextlib
import numpy as np
import concourse.bass as bass
import concourse.mybir as mybir

F32 = mybir.dt.float32
BF16 = mybir.dt.bfloat16
AF = mybir.ActivationFunctionType
ALU = mybir.AluOpType
AX = mybir.AxisListType

NDS = 16
EP = 3000


class Sched:
    def __init__(self):
        self.streams = {e: [] for e in ('pe', 'act', 'dve', 'pool', 'sp')}
        self.cnt = {e: 0 for e in ('pe', 'act', 'dve', 'pool')}
        self.known = {e: {} for e in self.streams}
        self.snap = {}
        self.acc = {}
        self.dma_n = 0
        self.dma_cnt = [0] * NDS
        self.tracked = set()

    def track(self, t):
        self.tracked.add(t.name)
        return t

    def _box(self, ap):
        t = ap.tensor
        row = 1
        for s in t.shape[1:]:
            row *= s
        if t.name.startswith('pb') or t.name.startswith('pt'):
            return (0, 128, 0, row)
        off = ap.offset
        p0 = off // row
        f0 = off % row
        apl = ap.ap
        ps, pc = apl[0]
        p1 = p0 + (pc - 1) * (ps // row if ps else 0) + 1
        ext = 1
        for s, c in apl[1:]:
            ext += (c - 1) * abs(s)
        return (p0, p1, f0, f0 + ext)

    def _deps(self, ap, is_write, eng, deps):
        name = ap.tensor.name
        if name not in self.tracked:
            return
        b = self._box(ap)
        psum = name.startswith('pb') or name.startswith('pt')
        for r in self.acc.get(name, ()):
            if r[0] >= b[1] or b[0] >= r[1] or r[2] >= b[3] or b[2] >= r[3]:
                continue
            if not is_write and not r[5]:
                if not (psum and r[6] != eng):
                    continue
            if r[6] == eng and eng != 'sp':
                if eng == 'pe':
                    continue
                if is_write:
                    continue
            deps.add(r[4])

    def _record(self, ap, is_write, eng, tok):
        name = ap.tensor.name
        if name not in self.tracked:
            return
        b = self._box(ap)
        lst = self.acc.setdefault(name, [])
        if is_write:
            lst[:] = [r for r in lst if not (b[0] <= r[0] and r[1] <= b[1] and b[2] <= r[2] and r[3] <= b[3])]
            lst.append([b[0], b[1], b[2], b[3], tok, True, eng])
        else:
            for r in lst:
                if (not r[5]) and r[6] == eng and b[0] <= r[0] and r[1] <= b[1] and b[2] <= r[2] and r[3] <= b[3]:
                    r[0], r[1], r[2], r[3], r[4] = b[0], b[1], b[2], b[3], tok
                    return
            lst.append([b[0], b[1], b[2], b[3], tok, False, eng])

    def op(self, eng, fn, reads=(), writes=(), signal=True):
        deps = set()
        for ap in reads:
            self._deps(ap, False, eng, deps)
        for ap in writes:
            self._deps(ap, True, eng, deps)
        if eng == 'sp':
            j = self.dma_n % NDS
            self.dma_n += 1
            if self.dma_cnt[j] > 0:
                deps.add(('d%d' % j, self.dma_cnt[j]))
            self.dma_cnt[j] += 1
            tok = ('d%d' % j, self.dma_cnt[j])
            signal = True
        else:
            tok = (eng, self.cnt[eng] + 1)
            if signal:
                self.cnt[eng] += 1
        kn = self.known[eng]
        need = {}
        for te, v in deps:
            if kn.get(te, 0) >= v:
                continue
            if v > need.get(te, 0):
                need[te] = v
        waits = []
        for te, v in need.items():
            if kn.get(te, 0) >= v:
                continue
            waits.append((te, v))
            sn = self.snap.get((te, v))
            if sn:
                for k2, v2 in sn.items():
                    if kn.get(k2, 0) < v2:
                        kn[k2] = v2
            kn[te] = v
        self.streams[eng].append((waits, fn, tok if signal else None))
        if signal:
            self.snap[tok] = dict(kn)
        for ap in reads:
            self._record(ap, False, eng, tok)
        for ap in writes:
            self._record(ap, True, eng, tok)
        return tok

    def finish(self):
        waits = [('d%d' % j, self.dma_cnt[j]) for j in range(NDS) if self.dma_cnt[j] > 0]
        self.streams['sp'].append((waits, None, None))

    def emit(self, nc, stack):
        sems = {}

        def sem_of(te, v):
            if te[0] == 'd' and te[1:].isdigit():
                key = te
                val = 16 * v
            else:
                ep = (v - 1) // EP
                key = '%s_%d' % (te, ep)
                val = v - ep * EP
            if key not in sems:
                sems[key] = stack.enter_context(nc.semaphore('s_' + key))
            return sems[key], val

        for eng, st in self.streams.items():
            for waits, fn, tok in st:
                for te, v in waits:
                    sem_of(te, v)
                if tok:
                    sem_of(*tok)
        block = stack.enter_context(nc.Block())
        streams = self.streams

        def run(eng, e):
            for waits, fn, tok in streams[eng]:
                for te, v in waits:
                    s, val = sem_of(te, v)
                    e.wait_ge(s, val)
                if fn is None:
                    continue
                inst = fn(e)
                if tok:
                    s, _ = sem_of(*tok)
                    inst.then_inc(s, 16 if eng == 'sp' else 1)

        @block.tensor
        def _(e):
            run('pe', e)

        @block.scalar
        def _(e):
            run('act', e)

        @block.vector
        def _(e):
            run('dve', e)

        @block.gpsimd
        def _(e):
            run('pool', e)

        @block.sync
        def _(e):
            run('sp', e)
        return sems


class Ops:
    def __init__(self, S):
        self.S = S

    def mm(self, out, lhsT, rhs, start=True, stop=True, sig=True):
        self.S.op('pe', lambda e: e.matmul(out, lhsT, rhs, start=start, stop=stop),
                  reads=[lhsT, rhs], writes=[out], signal=sig)

    def tr(self, out, in_, ident, sig=True):
        self.S.op('pe', lambda e: e.transpose(out, in_, ident), reads=[in_, ident], writes=[out], signal=sig)

    def act(self, out, in_, func, bias=None, scale=None, accum=None, eng='act'):
        reads = [in_]
        kw = {}
        if bias is not None:
            kw['bias'] = bias
            if not isinstance(bias, (int, float)):
                reads.append(bias)
        if scale is not None:
            kw['scale'] = scale
            if not isinstance(scale, (int, float)):
                reads.append(scale)
        writes = [out]
        if accum is not None:
            kw['accum_out'] = accum
            writes.append(accum)
        self.S.op('act', lambda e: e.activation(out, in_, func, **kw), reads=reads, writes=writes)

    def tt(self, eng, out, a, b, op):
        self.S.op(eng, lambda e: e.tensor_tensor(out, a, b, op), reads=[a, b], writes=[out])

    def ts(self, eng, out, a, s1, op0, s2=None, op1=None):
        reads = [a]
        if not isinstance(s1, (int, float)):
            reads.append(s1)
        if s2 is not None and not isinstance(s2, (int, float)):
            reads.append(s2)
        if op1 is None:
            self.S.op(eng, lambda e: e.tensor_scalar(out, a, s1, None, op0), reads=reads, writes=[out])
        else:
            self.S.op(eng, lambda e: e.tensor_scalar(out, a, s1, s2, op0, op1), reads=reads, writes=[out])

    def stt(self, eng, out, a, s, b, op0, op1):
        reads = [a, b]
        if not isinstance(s, (int, float)):
            reads.append(s)
        self.S.op(eng, lambda e: e.scalar_tensor_tensor(out, a, s, b, op0, op1), reads=reads, writes=[out])

    def cp(self, eng, out, a):
        if eng == 'act':
            self.S.op('act', lambda e: e.copy(out, a), reads=[a], writes=[out])
        else:
            self.S.op(eng, lambda e: e.tensor_copy(out, a), reads=[a], writes=[out])

    def memset(self, eng, out, v):
        self.S.op(eng, lambda e: e.memset(out, v), reads=[], writes=[out])

    def scan(self, out, d0, d1, init, op0, op1):
        self.S.op('dve', lambda e: e.tensor_tensor_scan(out, d0, d1, init, op0, op1), reads=[d0, d1], writes=[out])

    def red(self, out, a, op=ALU.add, axis=AX.X):
        self.S.op('dve', lambda e: e.tensor_reduce(out, a, axis, op), reads=[a], writes=[out])

    def recip(self, out, a):
        self.S.op('dve', lambda e: e.reciprocal(out, a), reads=[a], writes=[out])

    def dma(self, out, in_):
        self.S.op('sp', lambda e: e.dma_start(out=out, in_=in_), reads=[in_], writes=[out])

from concourse.bass_utils import run_bass_kernel_spmd

D_IN = 4744
CC = 0.5 * float(np.exp(-0.5))
EPS = 1e-6
GN_EPS = 64e-5
LN_EPS = 1e-6

_pc_names = [('MU', 13), ('W0', 4), ('A0', 4), ('KK', 4), ('KA', 4), ('RK', 4), ('GNW', 4), ('GNB', 4),
             ('CW', 32), ('CB', 8), ('LNW', 4), ('SKIP', 4), ('GAIN', 8), ('BSH', 8), ('BSC', 8), ('C', 8),
             ('BI', 1), ('BF', 1)]
_pc_der = [('OMM', 13), ('HW0', 4), ('HA0', 4), ('HKK', 4), ('NKK', 4), ('HKA', 4), ('OMHKA', 4), ('HCW', 32),
           ('HCB', 8), ('HLNW', 4), ('HBF', 1), ('G', 8), ('SH', 8)]
PCI = {}
_o = 0
for _n, _w in _pc_names:
    PCI[_n] = _o
    _o += _w
NPC_IN = _o
for _n, _w in _pc_der:
    PCI[_n] = _o
    _o += _w
NPC = _o


def host_consts():
    c = np.zeros((128, 1152), np.float32)
    p = np.arange(128)[:, None]
    f = np.arange(128)[None, :]
    c[:, 0:128] = (p == f)
    c[:, 128:256] = (f > p)
    c[:, 256:384] = (f >= p)
    c[:, 384:512] = (f < p)
    c[:, 512:640] = (p // 64 == f // 64)
    for h in range(4):
        c[h, 640 + h * 128:640 + (h + 1) * 128] = 1.0
    return c


def host_consts2():
    c = np.zeros((128, 896), np.float32)
    sidx = np.arange(128)[:, None]
    tidx = np.arange(128)[None, :]
    for k in range(7):
        b = 1 << k
        c[:, k * 128:(k + 1) * 128] = ((tidx // (2 * b) == sidx // (2 * b)) & (tidx % (2 * b) >= b) & (sidx % (2 * b) < b))
    return c


def host_pcol(inp, b):
    pc = np.zeros((128, NPC_IN), np.float32)

    def put(name, vec):
        v = np.asarray(vec, np.float32).reshape(-1, 128).T
        pc[:, PCI[name]:PCI[name] + v.shape[1]] = v
    put('MU', inp['mu_rwkv'][0])
    put('W0', inp['w_decay0'][0]); put('A0', inp['a0'][0]); put('KK', inp['k_k'][0]); put('KA', inp['k_a'][0])
    put('RK', inp['r_k'][0]); put('GNW', inp['rwkv_gn_w'][0]); put('GNB', inp['rwkv_gn_b'][0])
    put('CW', inp['mlstm_conv_w'][0].reshape(-1)); put('CB', inp['mlstm_conv_b'][0])
    put('LNW', inp['mlstm_ln_w'][0]); put('SKIP', inp['mlstm_skip'][0]); put('GAIN', inp['norm_gain'][0])
    put('BSH', inp['b_ada'][0][0:1024]); put('BSC', inp['b_ada'][0][1024:2048]); put('C', inp['c'][b])
    pc[0:4, PCI['BI']] = inp['mlstm_b_i'][0]
    pc[0:4, PCI['BF']] = inp['mlstm_b_f'][0]
    return pc


def build(NCH=32):
    nc = bass.Bass("TRN2", target_bir_lowering=False)
    T = NCH * 128
    x = nc.dram_tensor("x", [T, 1024], F32, kind="ExternalInput").ap()
    w_in = nc.dram_tensor("w_in", [1024, D_IN], F32, kind="ExternalInput").ap()
    w_out = nc.dram_tensor("w_out", [1024, 1024], F32, kind="ExternalInput").ap()
    w_ada = nc.dram_tensor("w_ada", [1024, 3072], F32, kind="ExternalInput").ap()
    lora = nc.dram_tensor("lora", [128, 512], F32, kind="ExternalInput").ap()
    pcol = nc.dram_tensor("pcol", [128, NPC_IN], F32, kind="ExternalInput").ap()
    cst = nc.dram_tensor("cst", [128, 1152], F32, kind="ExternalInput").ap()
    cst2 = nc.dram_tensor("cst2", [128, 896], F32, kind="ExternalInput").ap()
    bgate = nc.dram_tensor("bgate", [1, 1024], F32, kind="ExternalInput").ap()
    fgain = nc.dram_tensor("fgain", [1, 1024], F32, kind="ExternalInput").ap()
    y = nc.dram_tensor("y", [T, 1024], F32, kind="ExternalOutput").ap()
    import os as _os
    DBG = bool(_os.environ.get("DBG"))
    if DBG:
        dbg = nc.dram_tensor("dbg", [NCH, 128, 4096], F32, kind="ExternalOutput").ap()
    S = Sched()
    O = Ops(S)
    P = PCI
    with contextlib.ExitStack() as st:
        def sb(name, shape, dt=F32):
            return S.track(st.enter_context(nc.sbuf_tensor(name, shape, dt)))

        def ps(name, shape, dt=F32):
            return S.track(st.enter_context(nc.psum_tensor(name, shape, dt)))
        pb = [ps("pb%d" % i, [128, 512]) for i in range(6)]
        pt = [ps("pt%d" % i, [128, 1024], BF16) for i in range(2)]
        rot = [0]

        def nb():
            b = pb[rot[0] % 5]
            rot[0] += 1
            return b
        Wb = sb("Wb", [128, 8, D_IN], BF16)
        Wo = sb("Wo", [128, 8, 1024], BF16)
        loraB = sb("loraB", [128, 512], BF16)
        pc = sb("pc", [128, NPC])
        cB = sb("cB", [128, 2048], BF16)
        identB = cB[:, 0:128]; ms = cB[:, 128:256]; mTs = cB[:, 256:384]; mTi = cB[:, 384:512]
        bonesB = cB[:, 512:640]; selB = cB[:, 640:1152]
        mlev = [cB[:, 1152 + k * 128:1152 + (k + 1) * 128] for k in range(7)]

        def rep4(m2d):
            return m2d.unsqueeze(1).broadcast_to([128, 4, 128])

        def N4(t2d):
            return t2d.rearrange("p (h c) -> p h c", h=4)
        tmpg = sb("tmpg", [128, 1, 256]); fg_b3 = sb("fg_b", [128, 1, 1024])
        fg_b = fg_b3[:, 0, :]
        onesF = sb("onesF", [128, 128]); mhalf = sb("mhalf", [128, 8]); mone = sb("mone", [128, 8])
        arena = sb("arena", [128, 4096])
        xb = arena[:, 0:1024]; hres = arena[:, 1024:2048]; outb = arena[:, 2048:3072]; misc = arena[:, 3072:4096]
        ysq = arena[:, 3072:3584]; nsq = arena[:, 3584:4096]
        stg = [arena[:, 0:2048].rearrange("p (k n) -> p k n", k=8), arena[:, 2048:4096].rearrange("p (k n) -> p k n", k=8)]
        xsb = sb("xsb", [128, 1024], BF16)
        hnT = sb("hnT", [128, 8, 131], BF16)
        ssq = sb("ssq", [128, 8])
        A = sb("A", [128, 13, 128], BF16); cf = arena[:, 1024:2176]; Bt = sb("Bt", [128, 1, 129]); halo = sb("halo", [128, 13])
        lor = sb("lor", [128, 128], BF16)
        th = sb("th", [128, 1, 128]); tha = sb("tha", [128, 1, 128]); cum = sb("cum", [128, 1, 128])
        ET = sb("ET", [128, 1, 129]); Einv = sb("Einv", [128, 1, 128])
        sq = sb("sq", [128, 1, 128], BF16); rs = sb("rs", [128, 1, 128]); f1 = sb("f1", [128, 1, 128])
        kmod = sb("kmod", [128, 1, 128]); t1 = sb("t1", [128, 1, 128]); rk = sb("rk", [128, 1, 128], BF16)
        aT = [sb("aT%d" % i, [128, 8, 128], BF16) for i in range(2)]; bT = [sb("bT%d" % i, [128, 8, 128], BF16) for i in range(2)]
        kT = [sb("kT%d" % i, [128, 8, 128], BF16) for i in range(2)]; rT = [sb("rT%d" % i, [128, 8, 128], BF16) for i in range(2)]; bhT = sb("bhT", [128, 4, 128], BF16); khT = sb("khT", [128, 4, 128], BF16)
        vbf = sb("vbf", [128, 4, 128], BF16); bonus = [sb("bonus%d" % i, [128, 4, 128], BF16) for i in range(2)]; szr = [sb("szr%d" % i, [128, 4, 128], BF16) for i in range(2)]
        zh = sb("zh", [128, 1, 128]); thz = sb("thz", [128, 1, 128])
        WLs = [sb("WLs%d" % i, [128, 4]) for i in range(2)]
        btok = sb("btok", [128, 512], BF16); ktok = sb("ktok", [128, 512], BF16); vtok = sb("vtok", [128, 512], BF16)
        sAk = [sb("sAk%d" % i, [128, 512], BF16) for i in range(2)]; sRb = [sb("sRb%d" % i, [128, 512], BF16) for i in range(2)]; sRk = [sb("sRk%d" % i, [128, 512], BF16) for i in range(2)]
        Nb = [sb("Nb%d" % i, [128, 512], BF16) for i in range(2)]; Mbb = [sb("Mbb%d" % i, [128, 512], BF16) for i in range(2)]
        Gs = [sb("Gs%d" % i, [128, 512], BF16) for i in range(2)]
        MTb = [sb("MTb%d" % i, [128, 512], BF16) for i in range(2)]
        Xs = sb("Xs", [128, 512], BF16); SAs = sb("SAs", [128, 512], BF16)
        Sst = sb("Sst", [128, 4, 64]); Sbf = sb("Sbf", [128, 4, 64], BF16); Sd = sb("Sd", [128, 4, 64])
        gs = sb("gs", [128, 48]); yn = sb("yn", [128, 512], BF16); scm = yn; hnb = yn; y1 = sb("y1", [128, 1, 128])
        Rq = sb("Rq", [128, 8, 131], BF16); acc = sb("acc", [128, 2, 128]); thq = sb("thq", [128, 512], BF16)
        qc = [sb("qc%d" % i, [128, 4, 128], BF16) for i in range(2)]; loraF = arena[:, 2176:2688]; kc = sb("kc", [128, 4, 128], BF16)
        qt = [sb("qt%d" % i, [128, 4, 128], BF16) for i in range(2)]; kt_ = [sb("kt_%d" % i, [128, 4, 128], BF16) for i in range(2)]
        A1 = [sb("A1_%d" % i, [128, 4, 128], BF16) for i in range(2)]; szm = [sb("szm%d" % i, [128, 4, 128], BF16) for i in range(2)]; tho = sb("tho", [128, 1, 128])
        gsm = sb("gsm", [4, 5, 128]); gsb = sb("gsb", [4, 4, 128], BF16)
        e1L = [sb("e1L%d" % i, [128, 4]) for i in range(2)]
        vmtok = [sb("vmtok%d" % i, [128, 4, 129], BF16) for i in range(2)]; kmtok = sb("kmtok", [128, 512], BF16)
        Cst = sb("Cst", [128, 4, 129]); Cbf = sb("Cbf", [128, 4, 129], BF16); Cd = sb("Cd", [128, 2, 129])
        mst = sb("mst", [128, 12, 4]); y2 = sb("y2", [128, 1, 128])
        yT = sb("yT", [128, 8, 128], BF16)
        cact = sb("cact", [128, 24]); cactB = sb("cactB", [128, 8], BF16); cbB = Wo[:, :, 512:640]
        wab = [Wo[:, :, i * 256:(i + 1) * 256] for i in range(2)]

        def pcs(name, i=0, rows=slice(0, 128)):
            return pc[rows, P[name] + i:P[name] + i + 1]

        O.dma(pc[:, 0:NPC_IN], pcol)
        O.dma(cf[:], cst)
        O.dma(loraF[:], lora)
        O.dma(fg_b3[:], fgain.partition_broadcast(128))
        O.memset('dve', onesF[:], 1.0); O.memset('dve', mhalf[:], -0.5); O.memset('dve', mone[:], -1.0)
        O.memset('dve', halo[:], 0.0); O.memset('dve', Rq[:], 0.0); O.memset('dve', hnT[:], 0.0); O.memset('dve', Sst[:], 0.0)
        O.memset('dve', Sbf[:], 0.0); O.memset('dve', Cst[:], 0.0); O.memset('dve', Cbf[:], 0.0)
        O.memset('dve', ET[:], 1.0); O.memset('dve', vmtok[0][:], 1.0); O.memset('dve', vmtok[1][:], 1.0)
        for z_ in aT + bT + kT + rT:
            O.memset('dve', z_[:], 0.0)
        O.dma(arena[:, 0:896], cst2)
        gate_bf = xsb
        O.cp('dve', identB, cf[:, 0:128])
        O.cp('dve', ms, cf[:, 384:512])
        O.cp('dve', mTs, cf[:, 128:256])
        O.cp('dve', mTi, cf[:, 256:384])
        O.cp('dve', cB[:, 1152:2048], arena[:, 0:896])
        O.cp('dve', bonesB, cf[:, 512:640])
        O.cp('dve', selB, cf[:, 640:1152])
        O.cp('dve', loraB[:], loraF[:])

        def dts(dst, src, n, m, a):
            O.ts('dve', pc[:, P[dst]:P[dst] + n], pc[:, P[src]:P[src] + n], m, ALU.mult, a, ALU.add)
        dts('OMM', 'MU', 13, -1.0, 1.0); dts('HW0', 'W0', 4, 0.5, 0.0); dts('HA0', 'A0', 4, 0.5, 0.0)
        dts('HKK', 'KK', 4, 0.5, 0.0); dts('NKK', 'KK', 4, -1.0, 0.0); dts('HKA', 'KA', 4, 0.5, 0.0)
        dts('OMHKA', 'KA', 4, -0.5, 1.0); dts('HCW', 'CW', 32, 0.5, 0.0); dts('HCB', 'CB', 8, 0.5, 0.0)
        dts('HLNW', 'LNW', 4, 0.5, 0.0); dts('HBF', 'BF', 1, 0.5, 0.0)
        O.act(cact[:, 0:8], pc[:, P['C']:P['C'] + 8], AF.Tanh, scale=0.5)
        O.ts('dve', cact[:, 8:16], pc[:, P['C']:P['C'] + 8], 0.5, ALU.mult)
        O.stt('dve', cact[:, 16:24], cact[:, 0:8], 1.0, cact[:, 8:16], ALU.add, ALU.mult)
        O.cp('dve', cactB[:], cact[:, 16:24])
        for kt in range(8):
            O.cp('dve', cbB[:, kt, :], cact[:, 16 + kt:17 + kt].broadcast_to([128, 128]))
        wada_v = w_ada.rearrange("(kt p) n -> p kt n", p=128)
        pcp = pb[5]
        for j in range(12):
            sg = stg[j % 2]
            wb_ = wab[j % 2]
            O.dma(sg[:, :, 0:256], wada_v[:, :, j * 256:(j + 1) * 256])
            O.cp(('act', 'dve')[j % 2], wb_[:], sg[:, :, 0:256])
            if j < 8:
                for sub in range(2):
                    ft = j * 2 + sub
                    for kt in range(8):
                        O.mm(pcp[:, ft:ft + 1], wb_[:, kt, sub * 128:(sub + 1) * 128], cactB[:, kt:kt + 1],
                             start=(kt == 0), stop=(kt == 7), sig=(kt == 7))
            else:
                pg = nb()
                for kt in range(8):
                    O.mm(pg[:, 0:256], cbB[:, kt, :], wb_[:, kt, :], start=(kt == 0), stop=(kt == 7), sig=(kt == 7))
                O.dma(tmpg[:], bgate[:, (j - 8) * 256:(j - 7) * 256].partition_broadcast(128))
                O.tt('dve', gate_bf[:, (j - 8) * 256:(j - 7) * 256], pg[:, 0:256], tmpg[:, 0, :], ALU.add)
        O.tt('dve', pc[:, P['SH']:P['SH'] + 8], pcp[:, 0:8], pc[:, P['BSH']:P['BSH'] + 8], ALU.add)
        O.tt('dve', cact[:, 0:8], pcp[:, 8:16], pc[:, P['BSC']:P['BSC'] + 8], ALU.add)
        O.stt('dve', pc[:, P['G']:P['G'] + 8], cact[:, 0:8], 1.0, pc[:, P['GAIN']:P['GAIN'] + 8], ALU.add, ALU.mult)
        win_v = w_in.rearrange("(kt p) n -> p kt n", p=128)
        wout_v = w_out.rearrange("(kt p) n -> p kt n", p=128)
        engs = ('act', 'dve', 'pool')
        j = 0
        for c0 in range(0, D_IN, 256):
            w = min(256, D_IN - c0)
            sg = stg[j % 2]
            O.dma(sg[:, :, 0:w], win_v[:, :, c0:c0 + w])
            O.cp(engs[j % 3], Wb[:, :, c0:c0 + w], sg[:, :, 0:w])
            j += 1
        for c0 in range(0, 1024, 256):
            sg = stg[j % 2]
            O.dma(sg[:, :, 0:256], wout_v[:, :, c0:c0 + 256])
            O.cp(engs[j % 3], Wo[:, :, c0:c0 + 256], sg[:, :, 0:256])
            j += 1
        for j2 in range(8):
            O.tt(('dve', 'pool')[j2 % 2], Wo[:, j2, :], Wo[:, j2, :], gate_bf[:], ALU.mult)

        def hs(tile, h):
            return tile[:, h, :]

        tiles = [(128 * i, ('lerp', i)) for i in range(13)] + [(1664 + 128 * i, ('zr', i)) for i in range(4)] \
            + [(2176 + 128 * i, ('mqk', i)) for i in range(4)] + [(2688 + 128 * i, ('mqk', 4 + i)) for i in range(4)] \
            + [(3712 + 128 * i, ('mo', i)) for i in range(4)] + [(4232 + 128 * i, ('zm', i)) for i in range(4)]

        poolM = [0]
        poolS = [0]

        def nbM():
            b_ = pb[poolM[0] % 4]
            poolM[0] += 1
            return b_

        def nbS():
            b_ = pb[4 + poolS[0] % 2]
            poolS[0] += 1
            return b_
        ptM = pt[0]
        ptS = pt[1]

        def gen_PB(c):
            t0 = c * 128
            par = c % 2
            if c == 0:
                O.dma(xb, x[t0:t0 + 128, :])
            O.act(xsb[:], xb, AF.Square, accum=ssq[:, 0:1])
            yield
            O.ts('dve', ssq[:, 1:2], ssq[:, 0:1], 1.0 / 1024, ALU.mult, EPS, ALU.add)
            O.tt('pool', ssq[:, 2:3], ssq[:, 1:2], mhalf[:, 0:1], ALU.pow)
            yield
            O.ts('dve', xsb[:], xb, ssq[:, 2:3], ALU.mult)
            if c + 1 < NCH:
                O.dma(xb, x[t0 + 128:t0 + 256, :])
            yield
            O.cp('dve', hnT[:, :, 0:3], hnT[:, :, 128:131])
            for half in range(2):
                for q in range(4):
                    kt = half * 4 + q
                    O.tr(ptS[:, q * 128:(q + 1) * 128], xsb[:, kt * 128:(kt + 1) * 128], identB, sig=(q == 3))
                yield
                for q in range(4):
                    kt = half * 4 + q
                    O.act(hnT[:, kt, 3:131], ptS[:, q * 128:(q + 1) * 128], AF.Identity,
                          bias=pcs('SH', kt), scale=pcs('G', kt))
                yield

            def handle(hd, Pp):
                kind, i = hd
                if kind == 'lerp':
                    bt = Bt[:, 0, :]
                    O.act(bt[:, 0:128], Pp[:, 0:128], AF.Identity, scale=pcs('MU', i))
                    O.stt('dve', A[:, i, :], Pp[:, 1:129], pcs('OMM', i), bt[:, 0:128], ALU.mult, ALU.add)
                elif kind in ('zr', 'zm'):
                    dst = szr[par] if kind == 'zr' else szm[par]
                    O.act(zh[:, 0, :], Pp, AF.Identity, scale=0.5)
                    O.act(thz[:, 0, :], Pp, AF.Tanh, scale=0.5)
                    O.ts('pool', thz[:, 0, :], thz[:, 0, :], 1.0, ALU.add, 1.0, ALU.mult)
                    O.tt('pool', dst[:, i, :], thz[:, 0, :], zh[:, 0, :], ALU.mult)
                elif kind == 'mo':
                    O.act(tho[:, 0, :], Pp, AF.Tanh, scale=0.5)
                    O.ts('pool', A1[par][:, i, :], tho[:, 0, :], 1.0, ALU.add, pcs('HLNW', i), ALU.mult)
                elif kind == 'mqk':
                    O.cp('act', Rq[:, i, :], Pp[:, 0:131])
            GW = 2
            deferred = []
            for gi in range(0, len(tiles), GW):
                bank = nbS()
                grp = tiles[gi:gi + GW]
                def win(hd):
                    return {'lerp': (2, 129), 'mqk': (0, 131)}.get(hd[0], (3, 128))
                for q, (col0, hd) in enumerate(grp):
                    w0, wn = win(hd)
                    for kt in range(8):
                        O.mm(bank[:, q * 131:q * 131 + wn], Wb[:, kt, col0:col0 + 128], hnT[:, kt, w0:w0 + wn],
                             start=(kt == 0), stop=(kt == 7), sig=(kt == 7 and q == len(grp) - 1))
                yield
                ready, deferred = deferred, []
                for fn_ in ready:
                    fn_()
                for q, (col0, hd) in enumerate(grp):
                    w0, wn = win(hd)
                    handle(hd, bank[:, q * 131:q * 131 + wn])
                yield
            for fn_ in deferred:
                fn_()
            deferred = []
            yield
            T4 = thq[:].rearrange("p (a b) -> p a b", a=4)
            for gq, dst4 in enumerate((qc[par], kc)):
                D4 = dst4[:]

                def wb(col):
                    return pc[:, col:col + 4].unsqueeze(2).broadcast_to([128, 4, 128])
                O.tt('dve', D4, Rq[:, gq * 4:gq * 4 + 4, 0:128], wb(P['HCW'] + gq * 4), ALU.mult)
                O.tt('dve', D4, D4, wb(P['HCB'] + gq * 4), ALU.add)
                for tap in range(1, 4):
                    O.tt('dve', T4, Rq[:, gq * 4:gq * 4 + 4, tap:tap + 128], wb(P['HCW'] + tap * 8 + gq * 4), ALU.mult)
                    O.tt('dve', D4, D4, T4, ALU.add)
                yield
                d2 = dst4[:].rearrange("p a b -> p (a b)")
                O.act(thq[:], d2, AF.Tanh)
                yield
                O.stt('dve', d2, thq[:], 1.0, d2, ALU.add, ALU.mult)
                yield
            bank = nbS()
            for gg, col0 in enumerate((4224, 4228)):
                for kt in range(8):
                    O.mm(bank[0:4, gg * 128:(gg + 1) * 128], Wb[:, kt, col0:col0 + 4], hnT[:, kt, 3:131],
                         start=(kt == 0), stop=(kt == 7), sig=(kt == 7 and gg == 1))
            yield
            ei, thf, E1, rE1, E2 = [gsm[:, k, :] for k in range(5)]
            sf = thf
            E1h, E1l, E2h, E2l = [gsb[:, k, :] for k in range(4)]
            O.act(ei, bank[0:4, 0:128], AF.Exp, bias=pcs('BI', 0, slice(0, 4)))
            O.act(thf, bank[0:4, 128:256], AF.Tanh, bias=pcs('HBF', 0, slice(0, 4)), scale=0.5)
            yield
            O.ts('dve', sf, thf, 0.5, ALU.mult, 0.5, ALU.add)
            O.scan(E1, sf, onesF[0:4, :], 1.0, ALU.mult, ALU.mult)
            O.recip(rE1, E1)
            O.stt('dve', E2, ei, 128.0 ** -0.5, rE1, ALU.mult, ALU.mult)
            O.cp('dve', E1h, E1); O.tt('dve', E1l, E1, E1h, ALU.subtract)
            O.cp('dve', E2h, E2); O.tt('dve', E2l, E2, E2h, ALU.subtract)
            yield
            bkb = nbS(); bkc = nbS()
            for h in range(4):
                O.mm(bkb[:, h * 128:(h + 1) * 128], selB[0:4, h * 128:(h + 1) * 128], E1h, start=True, stop=False, sig=False)
                O.mm(bkb[:, h * 128:(h + 1) * 128], selB[0:4, h * 128:(h + 1) * 128], E1l, start=False, stop=True, sig=(h == 3))
            for h in range(4):
                O.mm(bkc[:, h * 128:(h + 1) * 128], selB[0:4, h * 128:(h + 1) * 128], E2h, start=True, stop=False, sig=False)
                O.mm(bkc[:, h * 128:(h + 1) * 128], selB[0:4, h * 128:(h + 1) * 128], E2l, start=False, stop=True, sig=(h == 3))
            yield
            for h in range(4):
                O.cp('dve', e1L[par][:, h:h + 1], bkb[:, h * 128 + 127:h * 128 + 128])
                O.tt('dve', qt[par][:, h, :], qc[par][:, h, :], bkb[:, h * 128:(h + 1) * 128], ALU.mult)
                O.tt('dve', kt_[par][:, h, :], kc[:, h, :], bkc[:, h * 128:(h + 1) * 128], ALU.mult)
            yield
            bank = nbS()
            for kt in range(8):
                O.mm(bank[:, 0:512], hnT[:, kt, 3:131], Wb[:, kt, 3200:3712], start=(kt == 0), stop=(kt == 7), sig=(kt == 7))
            yield
            O.cp('act', vmtok[par][:, :, 0:128], bank[:, 0:512].rearrange("p (h d) -> p h d", h=4))
            yield

        def gen_C1(c):
            par = c % 2
            O.act(lor[0:64, :], A[0:64, 12, :], AF.Tanh)
            O.cp('dve', lor[64:128, :], A[64:128, 12, :])
            yield
            for i in range(4):
                sl = slice(i * 128, (i + 1) * 128)
                bkA = nbS(); bkB = nbS()
                bzs = bkA[:, 0:128]; bsss = bkA[:, 128:256]; bbns = bkA[:, 256:384]; bas = bkB[:, 0:128]
                O.mm(bzs, loraB[0:64, sl], lor[0:64, :])
                O.mm(bas, loraB[64:128, sl], lor[64:128, :])
                Ak = A[:, 4 + i, :]; Ar = A[:, i, :]; Av = A[:, 8 + i, :]
                O.act(sq[:, 0, :], Ak, AF.Square, scale=pcs('KK', i))
                O.cp('act', vbf[:, i, :], Av)
                yield
                O.act(th[:, 0, :], bzs, AF.Tanh, bias=pcs('HW0', i), scale=0.5)
                O.act(tha[:, 0, :], bas, AF.Tanh, bias=pcs('HA0', i), scale=0.5)
                O.mm(bsss, bonesB, sq[:, 0, :])
                yield
                O.scan(cum[:, 0, :], th[:, 0, :], onesF[:], 0.0, ALU.add, ALU.add)
                O.ts('pool', f1[:, 0, :], tha[:, 0, :], pcs('HKA', i), ALU.mult, pcs('OMHKA', i), ALU.add)
                O.tt('pool', kmod[:, 0, :], Ak, f1[:, 0, :], ALU.mult)
                O.ts('pool', t1[:, 0, :], tha[:, 0, :], pcs('HKK', i), ALU.mult, pcs('HKK', i), ALU.add)
                O.tt('pool', t1[:, 0, :], t1[:, 0, :], Ak, ALU.mult)
                O.ts('dve', rs[:, 0, :], bsss, 1e-24, ALU.max)
                O.recip(rs[:, 0, :], rs[:, 0, :])
                yield
                O.act(ET[:, 0, 1:129], cum[:, 0, :], AF.Exp, scale=-CC)
                O.act(Einv[:, 0, :], cum[:, 0, :], AF.Exp, scale=CC)
                yield
                O.stt('dve', rk[:, 0, :], Ar, pcs('RK', i), kmod[:, 0, :], ALU.mult, ALU.mult)
                O.tt('dve', t1[:, 0, :], t1[:, 0, :], rs[:, 0, :], ALU.mult)
                O.mm(bbns, bonesB, rk[:, 0, :])
                yield
                WL = ET[:, 0, 128:129]
                O.stt('dve', bhT[:, i, :], t1[:, 0, :], WL, Einv[:, 0, :], ALU.mult, ALU.mult)
                O.stt('dve', khT[:, i, :], kmod[:, 0, :], WL, Einv[:, 0, :], ALU.mult, ALU.mult)
                for p_ in range(2):
                    rw = slice(p_ * 64, p_ * 64 + 64)
                    hh = 2 * i + p_
                    O.tt('dve', bT[par][rw, hh, :], t1[rw, 0, :], Einv[rw, 0, :], ALU.mult)
                    O.stt('dve', aT[par][rw, hh, :], A[rw, 4 + i, :], pc[rw, P['NKK'] + i:P['NKK'] + i + 1], ET[rw, 0, 0:128],
                          ALU.mult, ALU.mult)
                    O.tt('pool', kT[par][rw, hh, :], kmod[rw, 0, :], Einv[rw, 0, :], ALU.mult)
                    O.tt('pool', rT[par][rw, hh, :], A[rw, i, :], ET[rw, 0, 1:129], ALU.mult)
                O.tt('dve', bonus[par][:, i, :], bbns, Av, ALU.mult)
                O.cp('dve', WLs[par][:, i:i + 1], WL)
                yield

        def gen_C2(c):
            par = c % 2
            O.dma(hres, x[c * 128:(c + 1) * 128, :])
            for i in range(4):
                sl = slice(i * 128, (i + 1) * 128)
                O.tr(ptM[:, sl], bhT[:, i, :], identB, sig=False)
                O.tr(ptM[:, 512 + i * 128:512 + (i + 1) * 128], khT[:, i, :], identB, sig=(i == 3))
            yield
            O.cp('act', btok[:], ptM[:, 0:512]); O.cp('act', ktok[:], ptM[:, 512:1024])
            yield
            for i in range(4):
                sl = slice(i * 128, (i + 1) * 128)
                O.tr(ptM[:, sl], vbf[:, i, :], identB, sig=False)
                O.tr(ptM[:, 512 + i * 128:512 + (i + 1) * 128], kt_[par][:, i, :], identB, sig=(i == 3))
            yield
            O.cp('act', vtok[:], ptM[:, 0:512]); O.cp('act', kmtok[:], ptM[:, 512:1024])
            yield

        def gen_D(c):
            par = c % 2
            G2 = (0, 1)
            HD = [[4 * g + q for q in range(4)] for g in G2]

            def prod(lhs, rhs):
                bks = [nbM(), nbM()]
                for g in G2:
                    for q, h in enumerate(HD[g]):
                        sl = slice(q * 128, (q + 1) * 128)
                        O.mm(bks[g][:, sl], hs(lhs[par], h), hs(rhs[par], h), sig=(q == 3))
                return bks
            bN = prod(aT, bT)
            bNT = prod(bT, aT)
            yield
            for g in G2:
                O.tt('dve', N4(Nb[g][:]), N4(bN[g][:]), rep4(ms), ALU.mult)
                O.tt('dve', N4(MTb[g][:]), N4(bNT[g][:]), rep4(mlev[0]), ALU.mult)
            yield
            bAk = prod(kT, aT)
            bRb = prod(bT, rT)
            for g in G2:
                O.tt('dve', N4(MTb[g][:]), N4(MTb[g][:]), rep4(identB), ALU.add)
            yield
            for g in G2:
                O.cp('act', sAk[g][:], bAk[g][:])
                O.cp('act', sRb[g][:], bRb[g][:])
            yield
            for g in G2:
                O.tt('pool', N4(sAk[g][:]), N4(sAk[g][:]), rep4(mTs), ALU.mult)
                O.tt('pool', N4(sRb[g][:]), N4(sRb[g][:]), rep4(mTi), ALU.mult)
            bRk = prod(kT, rT)
            yield
            for g in G2:
                O.cp('act', sRk[g][:], bRk[g][:])
            yield
            for g in G2:
                O.tt('pool', N4(sRk[g][:]), N4(sRk[g][:]), rep4(mTi), ALU.mult)
            for lv in range(1, 7):
                b2 = [nbM(), nbM()]
                for g in G2:
                    for q in range(4):
                        sl = slice(q * 128, (q + 1) * 128)
                        O.tr(ptM[:, g * 512 + q * 128:g * 512 + (q + 1) * 128], MTb[g][:, sl], identB, sig=(q == 3))
                    for q in range(4):
                        sl = slice(q * 128, (q + 1) * 128)
                        O.mm(b2[g][:, sl], Nb[g][:, sl], MTb[g][:, sl], sig=(q == 3))
                yield
                O.cp('act', Mbb[0][:], ptM[:, 0:512])
                O.cp('act', Mbb[1][:], ptM[:, 512:1024])
                for g in G2:
                    O.tt('dve', N4(Gs[g][:]), N4(b2[g][:]), rep4(mlev[lv]), ALU.mult)
                yield
                b3 = [nbM(), nbM()]
                for g in G2:
                    for q in range(4):
                        sl = slice(q * 128, (q + 1) * 128)
                        O.mm(b3[g][:, sl], identB, MTb[g][:, sl], start=True, stop=False, sig=False)
                        O.mm(b3[g][:, sl], Mbb[g][:, sl], Gs[g][:, sl], start=False, stop=True, sig=(q == 3))
                yield
                O.cp('act', MTb[0][:], b3[0][:])
                O.cp('dve', MTb[1][:], b3[1][:])
                yield
            bX = nbM()
            for g in G2:
                for q, h in enumerate(HD[g]):
                    O.mm(bX[:, h * 64:(h + 1) * 64], hs(aT[par], h), Sbf[:, h // 2, :], start=True, stop=False, sig=False)
                    O.mm(bX[:, h * 64:(h + 1) * 64], sAk[g][:, q * 128:(q + 1) * 128], vtok[:, h * 64:(h + 1) * 64],
                         start=False, stop=True, sig=(h == 7))
            yield
            O.cp('act', Xs[:], bX[:, 0:512])
            yield
            bS = nbM()
            for g in G2:
                for q, h in enumerate(HD[g]):
                    O.mm(bS[:, h * 64:(h + 1) * 64], MTb[g][:, q * 128:(q + 1) * 128], Xs[:, h * 64:(h + 1) * 64], sig=(h == 7))
            yield
            O.cp('act', SAs[:], bS[:, 0:512])
            yield
            Yb = nbM()
            for g in G2:
                for q, h in enumerate(HD[g]):
                    yo = Yb[:, h * 64:(h + 1) * 64]
                    O.mm(yo, hs(rT[par], h), Sbf[:, h // 2, :], start=True, stop=False, sig=False)
                    O.mm(yo, sRb[g][:, q * 128:(q + 1) * 128], SAs[:, h * 64:(h + 1) * 64], start=False, stop=False, sig=False)
                    O.mm(yo, sRk[g][:, q * 128:(q + 1) * 128], vtok[:, h * 64:(h + 1) * 64], start=False, stop=True, sig=(h == 7))
            bSt = nbM()
            for i in range(4):
                sl = slice(i * 128, (i + 1) * 128)
                O.mm(bSt[:, sl], btok[:, sl], SAs[:, sl], start=True, stop=False, sig=False)
                O.mm(bSt[:, sl], ktok[:, sl], vtok[:, sl], start=False, stop=True, sig=(i == 3))
            O.tt('dve', Sd[:], Sst[:], WLs[par][:].unsqueeze(2).broadcast_to([128, 4, 64]), ALU.mult)
            yield
            Yv = Yb[:, 0:512].rearrange("p (h c) -> p h c", h=8)
            O.red(gs[:, 0:8], Yv)
            O.act(ysq, Yb[:, 0:512], AF.Square)
            for p_ in range(2):
                rows = slice(p_ * 64, p_ * 64 + 64)
                O.tt('dve', Sst[rows, :, :], N4(bSt[rows, 0:512])[:, :, p_ * 64:p_ * 64 + 64], Sd[rows, :, :], ALU.add)
            O.cp('act', Sbf[:], Sst[:])
            yield
            O.red(gs[:, 8:16], ysq.rearrange("p (h c) -> p h c", h=8))
            O.ts('dve', gs[:, 16:24], gs[:, 0:8], 1.0 / 64, ALU.mult)
            O.tt('dve', gs[:, 24:32], gs[:, 16:24], gs[:, 16:24], ALU.mult)
            O.stt('dve', gs[:, 32:40], gs[:, 8:16], 1.0 / 64, gs[:, 24:32], ALU.mult, ALU.subtract)
            O.ts('dve', gs[:, 32:40], gs[:, 32:40], GN_EPS, ALU.add)
            O.tt('pool', gs[:, 40:48], gs[:, 32:40], mhalf[:, 0:8], ALU.pow)
            yield
            yield
            ysq3 = ysq.rearrange("p (h c) -> p h c", h=8)
            O.tt('dve', ysq3, Yv, gs[:, 16:24].unsqueeze(2).broadcast_to([128, 8, 64]), ALU.subtract)
            O.tt('dve', yn[:].rearrange("p (h c) -> p h c", h=8), ysq3,
                 gs[:, 40:48].unsqueeze(2).broadcast_to([128, 8, 64]), ALU.mult)
            yield
            for i in range(4):
                O.tr(ptM[:, i * 128:(i + 1) * 128], yn[:, i * 128:(i + 1) * 128], identB, sig=(i == 3))
            yield
            def pcb(name):
                return pc[:, P[name]:P[name] + 4].unsqueeze(2).broadcast_to([128, 4, 128])
            yw = ysq.rearrange("p (h c) -> p h c", h=4)
            O.tt('dve', yw, N4(ptM[:, 0:512]), pcb('GNW'), ALU.mult)
            O.tt('dve', yw, yw, bonus[par][:], ALU.add)
            O.tt('dve', yw, yw, pcb('GNB'), ALU.add)
            O.tt('dve', yT[:, 0:4, :], yw, szr[par][:], ALU.mult)
            yield

        def gen_E(c):
            par = c % 2
            bsc = nbM()
            for h in range(4):
                sl = slice(h * 128, (h + 1) * 128)
                O.mm(bsc[:, sl], kt_[par][:, h, :], qt[par][:, h, :], sig=(h == 3))
            yield
            O.tt('dve', N4(scm[:]), N4(bsc[:]), rep4(mTi), ALU.mult)
            yield
            bH = [nbM(), nbM()]
            bU = [nbM(), nbM()]
            for h in range(4):
                o_ = (h % 2) * 129
                sl = slice(h * 128, (h + 1) * 128)
                O.mm(bH[h // 2][:, o_:o_ + 129], scm[:, sl], vmtok[par][:, h, :], start=True, stop=False, sig=False)
                O.mm(bH[h // 2][:, o_:o_ + 129], qt[par][:, h, :], Cbf[:, h, :], start=False, stop=True, sig=(h % 2 == 1))
            for h in range(4):
                o_ = (h % 2) * 129
                sl = slice(h * 128, (h + 1) * 128)
                O.mm(bU[h // 2][:, o_:o_ + 129], kmtok[:, sl], vmtok[par][:, h, :], sig=(h % 2 == 1))
            yield
            for b2 in range(2):
                Hv = bH[b2][:, 0:258].rearrange("p (h c) -> p h c", h=2)
                s2 = slice(2 * b2, 2 * b2 + 2)
                O.cp('dve', mst[:, 0, s2], Hv[:, :, 128])
                O.red(mst[:, 3, s2], Hv[:, :, 0:128])
                O.act(nsq.rearrange("p (h c) -> p h c", h=4)[:, s2, :], Hv[:, :, 0:128], AF.Square)
            yield
            for b2 in range(2):
                s2 = slice(2 * b2, 2 * b2 + 2)
                O.tt('dve', Cst[:, s2, :], bU[b2][:, 0:258].rearrange("p (h c) -> p h c", h=2), Cst[:, s2, :], ALU.add)
                O.tt('dve', Cst[:, s2, :], Cst[:, s2, :], e1L[par][:, s2].unsqueeze(2).broadcast_to([128, 2, 129]), ALU.mult)
            O.cp('act', Cbf[:], Cst[:])
            yield
            O.stt('dve', mst[:, 1, :], mst[:, 0, :], -1.0, mst[:, 0, :], ALU.mult, ALU.max)
            O.ts('dve', mst[:, 1, :], mst[:, 1, :], 1.0, ALU.max)
            O.recip(mst[:, 2, :], mst[:, 1, :])
            O.red(mst[:, 4, :], nsq.rearrange("p (h c) -> p h c", h=4))
            O.ts('dve', mst[:, 5, :], mst[:, 3, :], 1.0 / 128, ALU.mult)
            O.tt('dve', mst[:, 6, :], mst[:, 5, :], mst[:, 5, :], ALU.mult)
            O.stt('dve', mst[:, 7, :], mst[:, 4, :], 1.0 / 128, mst[:, 6, :], ALU.mult, ALU.subtract)
            O.tt('dve', mst[:, 8, :], mst[:, 2, :], mst[:, 2, :], ALU.mult)
            O.tt('dve', mst[:, 9, :], mst[:, 7, :], mst[:, 8, :], ALU.mult)
            O.ts('dve', mst[:, 9, :], mst[:, 9, :], LN_EPS, ALU.add)
            O.tt('pool', mst[:, 10, :], mst[:, 9, :], mhalf[:, 0:4], ALU.pow)
            yield
            O.tt('dve', mst[:, 11, :], mst[:, 2, :], mst[:, 10, :], ALU.mult)
            O.stt('dve', mst[:, 6, :], mst[:, 5, :], -1.0, mst[:, 11, :], ALU.mult, ALU.mult)
            yield
            for h in range(4):
                o_ = (h % 2) * 129
                O.act(hnb[:, h * 128:(h + 1) * 128], bH[h // 2][:, o_:o_ + 128], AF.Identity,
                      bias=mst[:, 6, h:h + 1], scale=mst[:, 11, h:h + 1])
            yield
            for h in range(4):
                O.tr(ptM[:, h * 128:(h + 1) * 128], hnb[:, h * 128:(h + 1) * 128], identB, sig=(h == 3))
            yield
            nw = nsq.rearrange("p (h c) -> p h c", h=4)
            yw2 = ysq.rearrange("p (h c) -> p h c", h=4)
            O.tt('dve', nw, N4(ptM[:, 0:512]), A1[par][:], ALU.mult)
            O.tt('dve', yw2, qc[par][:], pc[:, P['SKIP']:P['SKIP'] + 4].unsqueeze(2).broadcast_to([128, 4, 128]), ALU.mult)
            O.tt('dve', nw, nw, yw2, ALU.add)
            O.tt('dve', yT[:, 4:8, :], nw, szm[par][:], ALU.mult)
            yield

        def gen_F(c):
            t0 = c * 128
            for half in range(2):
                bo = nbM()
                hsl = slice(half * 512, (half + 1) * 512)
                for j2 in range(8):
                    O.mm(bo[:, 0:512], yT[:, j2, :], Wo[:, j2, hsl], start=(j2 == 0), stop=(j2 == 7), sig=(j2 == 7))
                yield
                O.tt('dve', hres[:, hsl], bo[:, 0:512], hres[:, hsl], ALU.add)
                yield
            O.act(misc, hres, AF.Square, accum=ssq[:, 4:5])
            yield
            O.ts('dve', ssq[:, 5:6], ssq[:, 4:5], 1.0 / 1024, ALU.mult, EPS, ALU.add)
            O.tt('pool', ssq[:, 6:7], ssq[:, 5:6], mhalf[:, 0:1], ALU.pow)
            yield
            O.stt('dve', outb, hres, ssq[:, 6:7], fg_b, ALU.mult, ALU.mult)
            O.dma(y[t0:t0 + 128, :], outb)
            if DBG:
                O.cp('dve', misc[:, 0:1024], yT[:].rearrange("p a b -> p (a b)"))
                O.dma(dbg[c, :, 0:1024], misc[:, 0:1024])
            yield

        def run_merged(items, counts=None):
            items = [[g, float(r), 0.0, 0, k] for k, (g, r) in enumerate(items) if g is not None]
            done = {}
            while items:
                for it in list(items):
                    it[2] += it[1]
                    while it[2] >= 1.0 and it in items:
                        it[2] -= 1.0
                        try:
                            next(it[0])
                            it[3] += 1
                        except StopIteration:
                            items.remove(it)
                            done[it[4]] = it[3]
            return done

        def chain(*gens):
            for g in gens:
                yield from g

        if not _os.environ.get('SKIPLOOP'):
            run_merged([(chain(gen_PB(0), gen_C1(0)), 1)])
            rates = (1.0, 1.0)
            for c in range(NCH):
                main = chain(gen_C2(c), gen_D(c), gen_E(c), gen_F(c))
                side = chain(gen_PB(c + 1), gen_C1(c + 1)) if c + 1 < NCH else None
                done = run_merged([(main, rates[0]), (side, rates[1])])
                if c == 0 and len(done) == 2:
                    n_main, n_side = done[0], done[1]
                    m = float(max(n_main, n_side))
                    rates = (n_main / m, n_side / m)
                    if _os.environ.get('DBGSTEPS'):
                        print('steps main/side', n_main, n_side, rates)
        S.finish()
        S.emit(nc, st)
    return nc


_NC_CACHE = {}


def kernel(**inputs):
    inp = {k: np.asarray(v) for k, v in inputs.items()}
    B, T, D = inp['x'].shape
    NCH = T // 128
    if NCH not in _NC_CACHE:
        _NC_CACHE[NCH] = build(NCH)
    nc = _NC_CACHE[NCH]
    cstv = host_consts()
    lora = np.ascontiguousarray(np.concatenate([inp['w_decay_up'][0], inp['w_icl_up'][0]], axis=0), np.float32)
    shared = {
        "w_in": np.ascontiguousarray(inp['w_in'][0], np.float32),
        "w_out": np.ascontiguousarray(inp['w_out'][0], np.float32),
        "w_ada": np.ascontiguousarray(inp['w_ada'][0], np.float32),
        "lora": lora, "cst": cstv, "cst2": host_consts2(),
        "bgate": np.ascontiguousarray(inp['b_ada'][0][2048:3072].reshape(1, 1024), np.float32),
        "fgain": np.ascontiguousarray(inp['final_gain'].reshape(1, 1024), np.float32),
    }
    in_maps = []
    for b in range(B):
        m = dict(shared)
        m["x"] = np.ascontiguousarray(inp['x'][b], np.float32)
        m["pcol"] = host_pcol(inp, b)
        in_maps.append(m)
    res = run_bass_kernel_spmd(nc, in_maps, core_ids=list(range(B)))
    global LAST_RES
    LAST_RES = res
    return np.stack([np.asarray(r["y"], np.float32) for r in res.results], axis=0)
```
